# Optimizing a Trainium2 kernel written in Bass

```python
import math
import jax, jax.numpy as jnp
from jax import lax
import numpy as np

D_MODEL = 1024
BATCH = 4
SEQ = 8192
DEPTH = 2

GRID_W = 64
HEAD_DIM = 64
DA_HEADS = 4
DA_QK = DA_HEADS * HEAD_DIM
DA_WIDTH = DA_HEADS * 2 * HEAD_DIM
NA_HEADS = 8
NA_WIDTH = NA_HEADS * HEAD_DIM
MIX_WIDTH = DA_WIDTH + NA_WIDTH
PROJ_SPLITS = [DA_QK, DA_QK, DA_QK, DA_QK, DA_WIDTH, NA_WIDTH, NA_WIDTH, NA_WIDTH]
PROJ_WIDTH = sum(PROJ_SPLITS)
NA_ROWS = 8
NA_COLS = 16
Q_BLOCK = 128
T5_BUCKETS = 32
T5_MAX_DIST = 128
DA_EPS = 1e-5
N_EXPERTS = 32
TOP_K = 4
D_FF = 1024
SWIGLU_ALPHA = 1.702
SWIGLU_LIMIT = 7.0
LN_EPS = 1e-5
DEEPNORM_ALPHA = (2.0 * DEPTH) ** 0.25
DEEPNORM_BETA = (8.0 * DEPTH) ** -0.25

kernel_name = "hymba_diffattn_natten_moe_deepnorm"


def layer_norm(x, g, b):
    x32 = x.astype(jnp.float32)
    mu = jnp.mean(x32, axis=-1, keepdims=True)
    var = jnp.mean(jnp.square(x32 - mu), axis=-1, keepdims=True)
    y = (x32 - mu) * lax.rsqrt(var + LN_EPS) * g.astype(jnp.float32) + b.astype(jnp.float32)
    return y.astype(x.dtype)


def t5_bucket(rel):
    half = T5_BUCKETS // 2
    max_exact = half // 2
    ret = (rel > 0).astype(jnp.int32) * half
    n = jnp.abs(rel)
    n_f = jnp.maximum(n, 1).astype(jnp.float32)
    large = max_exact + (jnp.log(n_f / max_exact) / math.log(T5_MAX_DIST / max_exact)
                         * (half - max_exact)).astype(jnp.int32)
    large = jnp.minimum(large, half - 1)
    return ret + jnp.where(n < max_exact, n, large)


def diff_attention(q1, q2, k1, k2, v, t5_table, lam, subln_g, lam_init):
    B, S = q1.shape[0], q1.shape[1]
    nb = S // Q_BLOCK
    scale = HEAD_DIM ** -0.5
    kpos = jnp.arange(S, dtype=jnp.int32)

    def blocks(t):
        return jnp.moveaxis(t.reshape(B, nb, Q_BLOCK, *t.shape[2:]), 1, 0)

    def one_block(args):
        q1b, q2b, start = args
        qpos = start + jnp.arange(Q_BLOCK, dtype=jnp.int32)
        rel = kpos[None, :] - qpos[:, None]
        bias = jnp.take(t5_table, t5_bucket(rel), axis=0)
        bias = jnp.transpose(bias, (2, 0, 1))[None].astype(jnp.float32)
        s1 = jnp.einsum('bqhd,bkhd->bhqk', q1b, k1).astype(jnp.float32) * scale + bias
        s2 = jnp.einsum('bqhd,bkhd->bhqk', q2b, k2).astype(jnp.float32) * scale + bias
        a = jax.nn.softmax(s1, axis=-1) - lam * jax.nn.softmax(s2, axis=-1)
        return jnp.einsum('bhqk,bkhe->bqhe', a.astype(v.dtype), v)

    starts = jnp.arange(nb, dtype=jnp.int32) * Q_BLOCK
    o = lax.map(one_block, (blocks(q1), blocks(q2), starts))
    o = jnp.moveaxis(o, 0, 1).reshape(B, S, DA_HEADS, 2 * HEAD_DIM)
    o32 = o.astype(jnp.float32)
    o32 = o32 * lax.rsqrt(jnp.mean(jnp.square(o32), axis=-1, keepdims=True) + DA_EPS)
    o32 = o32 * subln_g.astype(jnp.float32) * (1.0 - lam_init)
    return o32.reshape(B, S, DA_WIDTH).astype(v.dtype)


def neighbourhood_attention(q, k, v, rpb):
    B, S, H, d = q.shape
    rows = S // GRID_W
    kr = min(NA_ROWS, rows)
    scale = d ** -0.5
    qg = q.reshape(B, rows, GRID_W, H, d)
    kg = k.reshape(B, rows, GRID_W, H, d)
    vg = v.reshape(B, rows, GRID_W, H, d)
    col_start = np.clip(np.arange(GRID_W) - NA_COLS // 2, 0, GRID_W - NA_COLS)
    col_idx = (col_start[:, None] + np.arange(NA_COLS)[None, :]).astype(np.int32)
    col_off = (col_idx - np.arange(GRID_W)[:, None] + NA_COLS - 1).astype(np.int32)

    def one_row(r):
        rs = jnp.clip(r - kr // 2, 0, rows - kr)
        q_row = lax.dynamic_index_in_dim(qg, r, axis=1, keepdims=False)
        k_win = lax.dynamic_slice_in_dim(kg, rs, kr, axis=1)[:, :, col_idx]
        v_win = lax.dynamic_slice_in_dim(vg, rs, kr, axis=1)[:, :, col_idx]
        s = jnp.einsum('bqhd,brqchd->bhqrc', q_row, k_win).astype(jnp.float32) * scale
        row_off = rs + jnp.arange(kr, dtype=jnp.int32) - r + NA_ROWS - 1
        bias = rpb[:, row_off[None, :, None], col_off[:, None, :]]
        s = s + bias[None].astype(jnp.float32)
        p = jax.nn.softmax(s.reshape(B, H, GRID_W, kr * NA_COLS), axis=-1).reshape(s.shape)
        return jnp.einsum('bhqrc,brqchd->bqhd', p.astype(v.dtype), v_win)

    o = lax.map(one_row, jnp.arange(rows, dtype=jnp.int32))
    return jnp.moveaxis(o, 0, 1).reshape(B, S, H * d)


def moe(x, router_w, router_b, w_gu, b_gu, w_down, b_down):
    B, S, D = x.shape
    xt = x.reshape(B * S, D)
    logits = (xt @ router_w + router_b).astype(jnp.float32)
    top_v, top_i = lax.top_k(logits, TOP_K)
    gates = jax.nn.softmax(top_v, axis=-1)
    combine = jnp.sum(jax.nn.one_hot(top_i, N_EXPERTS, dtype=jnp.float32) * gates[..., None], axis=1)
    out = jnp.zeros_like(xt)
    for e in range(N_EXPERTS):
        h = xt @ w_gu[e] + b_gu[e]
        gate = jnp.minimum(h[:, :D_FF], SWIGLU_LIMIT)
        up = jnp.clip(h[:, D_FF:], -SWIGLU_LIMIT, SWIGLU_LIMIT)
        glu = gate * jax.nn.sigmoid(gate * SWIGLU_ALPHA)
        y = ((up + 1.0) * glu) @ w_down[e] + b_down[e]
        out = out + combine[:, e:e + 1].astype(y.dtype) * y
    return out.reshape(B, S, D)


def setup_inputs(seed: int = 0) -> dict:
    key = jax.random.key(seed)
    ks = jax.random.split(key, 20)
    f32 = jnp.float32
    nrm = lambda k, shape, s: jax.random.normal(k, shape, f32) * s
    return {
        "x": nrm(ks[0], (BATCH, SEQ, D_MODEL), 1.0),
        "w_in": nrm(ks[1], (DEPTH, D_MODEL, PROJ_WIDTH), D_MODEL ** -0.5),
        "w_out": nrm(ks[2], (DEPTH, MIX_WIDTH, D_MODEL), DEEPNORM_BETA * MIX_WIDTH ** -0.5),
        "lambda_q1": nrm(ks[3], (DEPTH, HEAD_DIM), 0.1),
        "lambda_k1": nrm(ks[4], (DEPTH, HEAD_DIM), 0.1),
        "lambda_q2": nrm(ks[5], (DEPTH, HEAD_DIM), 0.1),
        "lambda_k2": nrm(ks[6], (DEPTH, HEAD_DIM), 0.1),
        "subln_g": 1.0 + nrm(ks[7], (DEPTH, 2 * HEAD_DIM), 0.02),
        "t5_table": nrm(ks[8], (T5_BUCKETS, DA_HEADS), 0.1),
        "na_rpb": nrm(ks[9], (DEPTH, NA_HEADS, 2 * NA_ROWS - 1, 2 * NA_COLS - 1), 0.1),
        "ln1_g": 1.0 + nrm(ks[10], (DEPTH, D_MODEL), 0.02),
        "ln1_b": nrm(ks[11], (DEPTH, D_MODEL), 0.02),
        "router_w": nrm(ks[12], (DEPTH, D_MODEL, N_EXPERTS), D_MODEL ** -0.5),
        "router_b": nrm(ks[13], (DEPTH, N_EXPERTS), 0.01),
        "w_gate_up": nrm(ks[14], (DEPTH, N_EXPERTS, D_MODEL, 2 * D_FF), D_MODEL ** -0.5),
        "b_gate_up": nrm(ks[15], (DEPTH, N_EXPERTS, 2 * D_FF), 0.01),
        "w_down": nrm(ks[16], (DEPTH, N_EXPERTS, D_FF, D_MODEL), DEEPNORM_BETA * D_FF ** -0.5),
        "b_down": nrm(ks[17], (DEPTH, N_EXPERTS, D_MODEL), 0.01),
        "ln2_g": 1.0 + nrm(ks[18], (DEPTH, D_MODEL), 0.02),
        "ln2_b": nrm(ks[19], (DEPTH, D_MODEL), 0.02),
    }


def reference(x, w_in, w_out, lambda_q1, lambda_k1, lambda_q2, lambda_k2, subln_g, t5_table,
              na_rpb, ln1_g, ln1_b, router_w, router_b, w_gate_up, b_gate_up, w_down, b_down,
              ln2_g, ln2_b):
    B, S, D = x.shape
    split_at = list(np.cumsum(PROJ_SPLITS)[:-1])
    for l in range(DEPTH):
        h = x @ w_in[l]
        q1, q2, k1, k2, va, qn, kn, vn = jnp.split(h, split_at, axis=-1)
        hd = lambda t, nh: t.reshape(B, S, nh, t.shape[-1] // nh)
        lam_init = 0.8 - 0.6 * math.exp(-0.3 * l)
        lam = (jnp.exp(jnp.sum(lambda_q1[l].astype(jnp.float32) * lambda_k1[l].astype(jnp.float32)))
               - jnp.exp(jnp.sum(lambda_q2[l].astype(jnp.float32) * lambda_k2[l].astype(jnp.float32)))
               + lam_init)
        o_da = diff_attention(hd(q1, DA_HEADS), hd(q2, DA_HEADS), hd(k1, DA_HEADS), hd(k2, DA_HEADS),
                              hd(va, DA_HEADS), t5_table, lam, subln_g[l], lam_init)
        o_na = neighbourhood_attention(hd(qn, NA_HEADS), hd(kn, NA_HEADS), hd(vn, NA_HEADS), na_rpb[l])
        mix = jnp.concatenate([o_da, o_na], axis=-1) @ w_out[l]
        x = layer_norm(DEEPNORM_ALPHA * x + mix, ln1_g[l], ln1_b[l])
        ffn = moe(x, router_w[l], router_b[l], w_gate_up[l], b_gate_up[l], w_down[l], b_down[l])
        x = layer_norm(DEEPNORM_ALPHA * x + ffn, ln2_g[l], ln2_b[l])
    return x
```

```python
import contextlib
import numpy as np
import concourse.bass as bass
import concourse.mybir as mybir
from concourse.bass_utils import run_bass_kernel_spmd

F32 = mybir.dt.float32
BF16 = mybir.dt.bfloat16
AF = mybir.ActivationFunctionType
ALU = mybir.AluOpType
AX = mybir.AxisListType

ENGS = ("pe", "act", "dve", "pool", "sp")


class Op:
    __slots__ = ("eng", "fn", "deps", "needed", "val", "ctr", "step")

    def __init__(self, eng, fn, ctr, step):
        self.eng = eng
        self.fn = fn
        self.deps = []
        self.needed = False
        self.val = 0
        self.ctr = ctr
        self.step = step


class Builder:
    def __init__(self, nc, semstack=None, tag=""):
        self.nc = nc
        self.semstack = semstack
        self.tag = tag
        self.ops = {e: [] for e in ENGS}
        self.lastw = {}
        self.readers = {}
        self.ctr_ops = {}
        self.stack = contextlib.ExitStack()
        self.dma_slots = set()

    def sbuf(self, name, shape, dt):
        return self.stack.enter_context(self.nc.sbuf_tensor(self.tag + name, list(shape), dt))

    def psum(self, name, shape, dt=F32):
        return self.stack.enter_context(self.nc.psum_tensor(self.tag + name, list(shape), dt))

    def _record(self, op, reads, writes):
        e = op.eng
        deps = []
        for k in reads:
            w = self.lastw.get(k)
            if w is not None:
                deps.append(w)
        for k in writes:
            w = self.lastw.get(k)
            if w is not None:
                deps.append(w)
            for r in self.readers.get(k, ()):
                if r.eng == e and r.step == 1 and op.step == 1:
                    continue
                deps.append(r)
        for d in deps:
            if d is op:
                continue
            if e == "pe" and d.eng == "pe" and d.step == 1 and op.step == 1:
                continue
            d.needed = True
            if d.step == 16:
                op.deps.append((d.ctr, 16 * len(self.ctr_ops[d.ctr])))
            else:
                op.deps.append(d)
        for k in reads:
            self.readers.setdefault(k, []).append(op)
        for k in writes:
            self.lastw[k] = op
            self.readers[k] = []
        self.ops[e].append(op)
        self.ctr_ops.setdefault(op.ctr, []).append(op)
        return op

    def op(self, eng, fn, reads=(), writes=()):
        return self._record(Op(eng, fn, eng, 1), reads, writes)

    def dma(self, eng, slot, fn, reads=(), writes=()):
        self.dma_slots.add(slot)
        o = Op(eng, fn, "dma:" + slot, 16)
        o.needed = True
        return self._record(o, reads, writes)

    def emit(self, final_waits=()):
        nc = self.nc
        ctrs = list(self.ctr_ops.keys())
        sems = {}
        for c in ctrs:
            sems[c] = (self.semstack or self.stack).enter_context(
                nc.semaphore("s_" + self.tag + c.replace(":", "_")))
        for c, ops in self.ctr_ops.items():
            v = 0
            for o in ops:
                if o.needed:
                    v += o.step
                    o.val = v
                else:
                    o.val = v
        engmap = {"pe": "tensor", "act": "scalar", "dve": "vector", "pool": "gpsimd", "sp": "sync"}
        fin = {}
        for o in final_waits:
            fin[o.ctr] = max(fin.get(o.ctr, 0), o.val)
        self.nwaits = 0
        with nc.Block() as block:
            for e in ENGS:
                ops = self.ops[e]
                last = (e == "sp")
                if not ops and not last:
                    continue

                def body(eng, ops=ops, e=e, last=last):
                    known = {}
                    for o in ops:
                        need = {}
                        for d in o.deps:
                            c, v = d if isinstance(d, tuple) else (d.ctr, d.val)
                            if v > need.get(c, 0):
                                need[c] = v
                        for c, v in need.items():
                            if known.get(c, 0) < v:
                                eng.wait_ge(sems[c], v)
                                known[c] = v
                                self.nwaits += 1
                        ins = o.fn(eng)
                        if o.needed:
                            ins.then_inc(sems[o.ctr], o.step)
                    if last:
                        for c, v in fin.items():
                            eng.wait_ge(sems[c], v)

                getattr(block, engmap[e])(body)

    def close(self):
        self.stack.close()


D = 1024
FF = 1024
KC = 8
SBK = 512
ALPHA = (2.0 * 2) ** 0.25
LN_EPS = 1e-5


def _ln_feature_major(b, P, zt, src_keys, ones_f, ps_a, ps_b, tmp, gb, out_fn, tag):
    sq, mean, var, rstd = tmp["a"], tmp["b"], tmp["c"], tmp["d"]
    for fc in range(KC):
        b.op("pe", lambda e, fc=fc: e.matmul(ps_a[:, :], ones_f[:, :], zt[:, fc, :],
                                            start=(fc == 0), stop=(fc == KC - 1)),
             reads=[("z", fc)], writes=[ps_a.name] if fc in (0, KC - 1) else [])
    for fc in range(KC):
        b.op("act", lambda e, fc=fc: e.activation(out=sq[:, :], in_=zt[:, fc, :], func=AF.Square),
             reads=[("z", fc)], writes=[sq.name])
        b.op("pe", lambda e, fc=fc: e.matmul(ps_b[:, :], ones_f[:, :], sq[:, :],
                                            start=(fc == 0), stop=(fc == KC - 1)),
             reads=[sq.name], writes=[ps_b.name] if fc in (0, KC - 1) else [])
    b.op("dve", lambda e: e.tensor_scalar(out=mean[:, :], in0=ps_a[:, :], scalar1=1.0 / D, scalar2=None,
                                          op0=ALU.mult), reads=[ps_a.name], writes=[mean.name])
    b.op("dve", lambda e: e.tensor_tensor(out=var[:, :], in0=mean[:, :], in1=mean[:, :], op=ALU.mult),
         reads=[mean.name], writes=[var.name])
    b.op("dve", lambda e: e.scalar_tensor_tensor(out=var[:, :], in0=ps_b[:, :], scalar=1.0 / D, in1=var[:, :],
                                                 op0=ALU.mult, op1=ALU.subtract),
         reads=[ps_b.name, var.name], writes=[var.name])
    b.op("dve", lambda e: e.tensor_scalar(out=var[:, :], in0=var[:, :], scalar1=LN_EPS, scalar2=None,
                                          op0=ALU.add), reads=[var.name], writes=[var.name])
    b.op("act", lambda e: e.activation(out=rstd[:, :], in_=var[:, :], func=AF.Sqrt),
         reads=[var.name], writes=[rstd.name])
    b.op("dve", lambda e: e.reciprocal(out=rstd[:, :], in_=rstd[:, :]), reads=[rstd.name], writes=[rstd.name])
    for fc in range(KC):
        b.op("dve", lambda e, fc=fc: e.tensor_tensor(out=zt[:, fc, :], in0=zt[:, fc, :], in1=mean[:, :],
                                                     op=ALU.subtract),
             reads=[("z", fc), mean.name], writes=[("z", fc)])
        b.op("pool", lambda e, fc=fc: e.tensor_tensor(out=zt[:, fc, :], in0=zt[:, fc, :], in1=rstd[:, :],
                                                      op=ALU.mult),
             reads=[("z", fc), rstd.name], writes=[("z", fc)])
        b.op("dve", lambda e, fc=fc: e.tensor_scalar(out=zt[:, fc, :], in0=zt[:, fc, :],
                                                     scalar1=gb[:, fc:fc + 1], scalar2=gb[:, 8 + fc:9 + fc],
                                                     op0=ALU.mult, op1=ALU.add),
             reads=[("z", fc), gb.name], writes=[("z", fc)])
        out_fn(fc)


def moe_phase(nc, semstack, tag, io, T=4096, NE=32, TB=1024):
    NTB = T // TB
    NSB = TB // SBK

    og = io["og"]; xres = io["xres"]; w_out = io["w_out"]; ln1 = io["ln1"]; ln2 = io["ln2"]
    rw = io["rw"]; rb = io["rb"]; wgu = io["wgu"]; bguT = io["bguT"]; wdn = io["wdn"]; bdn = io["bdn"]
    ident = io["ident"]; smask = io["smask"]; y_f32 = io["y_f32"]; y_b16 = io.get("y_b16")

    b = Builder(nc, semstack, tag)
    NU = 12
    ring = [b.sbuf(f"ring{i}", [128, 1024], BF16) for i in range(NU)]
    wo = b.sbuf("wo", [128, KC, D], BF16)
    acc = b.sbuf("acc", [128, KC, TB], F32)
    x1b = b.sbuf("x1b", [128, KC, TB], BF16)
    actp = [[b.sbuf(f"act{p}_{i}", [128, KC, SBK], BF16) for i in range(2)] for p in range(2)]
    actb = actp[0]
    z = b.sbuf("z", [128, KC, SBK], F32)
    tg = [b.sbuf(f"tg{i}", [128, SBK], F32) for i in range(2)]
    ts = [b.sbuf(f"ts{i}", [128, SBK], F32) for i in range(2)]
    tu = [b.sbuf(f"tu{i}", [128, SBK], F32) for i in range(2)]
    cbt = [[b.sbuf(f"cb{p}_{i}", [128, SBK], F32) for i in range(2)] for p in range(2)]
    lnt = {"a": tg[0], "b": ts[0], "c": tu[0], "d": tg[1]}
    ln1_sb = b.sbuf("ln1_sb", [128, 16], F32); ln2_sb = b.sbuf("ln2_sb", [128, 16], F32)
    rw_sb = b.sbuf("rw_sb", [128, KC, NE], F32); rb_sb = b.sbuf("rb_sb", [128, NE], F32)
    bgu_sb = b.sbuf("bgu_sb", [128, NE * 16], F32)
    bdn_sb = b.sbuf("bdn_sb", [NE, D], F32)
    cand = actp[1][0]
    sm_sb = b.sbuf("sm_sb", [128, 2], F32)
    zb = [b.sbuf(f"zb{i}", [128, SBK], BF16) for i in range(2)]
    id_sb = b.sbuf("id_sb", [128, 128], F32)
    ones_f = b.sbuf("ones_f", [128, 128], F32)
    combT = b.sbuf("combT", [NE, TB], F32)
    lg = b.sbuf("lg", [128, NE], F32); ex = b.sbuf("ex", [128, NE], F32); msk = b.sbuf("msk", [128, NE], F32)
    mx8 = b.sbuf("mx8", [128, 8], F32); nmx = b.sbuf("nmx", [128, 1], F32); ssum = b.sbuf("ssum", [128, 1], F32)

    ps_g = [b.psum(f"ps_g{i}", [128, SBK]) for i in range(2)]
    ps_u = [b.psum(f"ps_u{i}", [128, SBK]) for i in range(2)]
    ps_d = [b.psum(f"ps_d{i}", [128, SBK]) for i in range(2)]
    ps_c = b.psum("ps_c", [128, SBK])
    ps_m = b.psum("ps_m", [128, SBK])

    def ld(eng, slot, dst, src, key):
        b.dma(eng, slot, lambda e: e.dma_start(out=dst, in_=src), writes=[key])

    ld("sp", "c0", ln1_sb[:, :], ln1[:, :], ln1_sb.name)
    ld("sp", "c0", ln2_sb[:, :], ln2[:, :], ln2_sb.name)
    ld("sp", "c0", rw_sb[:, :, :], rw.rearrange("(kc p) n -> p kc n", p=128), rw_sb.name)
    ld("sp", "c0", rb_sb[:, :], rb[0:1, :].broadcast_to([128, NE]), rb_sb.name)
    ld("sp", "c0", bgu_sb[:, :], bguT[:, :], bgu_sb.name)
    ld("sp", "c0", bdn_sb[:, :], bdn[:, :], bdn_sb.name)
    ld("sp", "c0", sm_sb[:, :], smask[:, :], sm_sb.name)
    ld("sp", "c0", id_sb[:, :], ident[:, :], id_sb.name)
    b.op("dve", lambda e: e.memset(ones_f[:, :], 1.0), writes=[ones_f.name])
    bgu3 = bgu_sb[:, :].rearrange("p (e c) -> p e c", c=16)
    b.op("dve", lambda e: e.tensor_scalar(out=bgu3[:, :, 8:16], in0=bgu3[:, :, 8:16], scalar1=1.0, scalar2=None,
                                          op0=ALU.add), reads=[bgu_sb.name], writes=[bgu_sb.name])

    units = []
    for tb_ in range(NTB):
        for e_ in range(NE):
            for fc_ in range(KC):
                units.append(wgu[e_, 2 * fc_]); units.append(wgu[e_, 2 * fc_ + 1])
                if e_ >= 1:
                    units.append(wdn[e_ - 1, fc_])
        for fo_ in range(KC):
            units.append(wdn[NE - 1, fo_])
    st = {"loaded": 0, "n": 0}

    def _ensure(upto):
        while st["loaded"] < min(upto, len(units)):
            i = st["loaded"]
            u = ring[i % NU]
            src = units[i]
            b.dma("pool", "ring%d" % (i % NU), lambda e, u=u, src=src: e.dma_start(out=u[:, :], in_=src),
                  writes=[u.name])
            st["loaded"] += 1

    def next_unit():
        _ensure(st["n"] + 1)
        u = ring[st["n"] % NU]
        st["n"] += 1
        return u[:, :].rearrange("p (k c) -> p k c", k=KC), u.name

    def unit_done():
        _ensure(st["n"] + NU)

    b.dma("pool", "wo", lambda e: e.dma_start(out=wo[:, :, :], in_=w_out.rearrange("(kc p) f -> p kc f", p=128)),
          writes=[wo.name])

    out_ops = []
    for tb in range(NTB):
        t0 = tb * TB
        _ensure(st["n"] + NU)
        for sb in range(NSB):
            c0 = t0 + sb * SBK
            ob = actb[sb % 2]
            b.dma("pool", "ob" + str(sb % 2), lambda e, ob=ob, c0=c0: e.dma_start(
                out=ob[:, :, :], in_=og[:, :, :, c0:c0 + SBK].rearrange("i r p t -> p (i r) t")),
                writes=[(ob.name, k_) for k_ in range(KC)])
            b.dma("pool", "cand", lambda e, c0=c0: e.dma_start(
                out=cand[:, :, :], in_=og[:, :, :, T + c0:T + c0 + SBK].rearrange("i r p t -> p (i r) t")),
                writes=[(cand.name, k_) for k_ in range(KC)])
            b.op("dve", lambda e, ob=ob: e.tensor_scalar(out=ob[:, :, :], in0=ob[:, :, :], scalar1=sm_sb[:, 0:1],
                                                         scalar2=None, op0=ALU.mult),
                 reads=[(ob.name, k_) for k_ in range(KC)] + [sm_sb.name], writes=[(ob.name, k_) for k_ in range(KC)])
            b.op("dve", lambda e, ob=ob: e.scalar_tensor_tensor(out=ob[:, :, :], in0=cand[:, :, :], scalar=sm_sb[:, 1:2],
                                                                in1=ob[:, :, :], op0=ALU.mult, op1=ALU.add),
                 reads=[(ob.name, k_) for k_ in range(KC)] + [(cand.name, k_) for k_ in range(KC)] + [sm_sb.name],
                 writes=[(ob.name, k_) for k_ in range(KC)])
            for fc in range(KC):
                b.dma("sp", "zin", lambda e, fc=fc, c0=c0: e.dma_start(
                    out=z[:, fc, :], in_=xres[fc * 128:(fc + 1) * 128, c0:c0 + SBK]), writes=[("z", fc)])
            for fc in range(KC):
                pp = ps_d[fc % 2]
                for kc in range(KC):
                    b.op("pe", lambda e, pp=pp, kc=kc, fc=fc, ob=ob, wo=wo: e.matmul(
                        pp[:, :], wo[:, kc, fc * 128:(fc + 1) * 128], ob[:, kc, :],
                        start=(kc == 0), stop=(kc == KC - 1)),
                        reads=[wo.name, (ob.name, kc)], writes=[pp.name] if kc in (0, KC - 1) else [])
                b.op("dve", lambda e, pp=pp, fc=fc: e.scalar_tensor_tensor(
                    out=z[:, fc, :], in0=z[:, fc, :], scalar=ALPHA, in1=pp[:, :], op0=ALU.mult, op1=ALU.add),
                    reads=[("z", fc), pp.name], writes=[("z", fc)])

            def after_ln1(fc, sb=sb):
                cs = slice(sb * SBK, (sb + 1) * SBK)
                b.op("act", lambda e: e.activation(out=x1b[:, fc, cs], in_=z[:, fc, :], func=AF.Copy),
                     reads=[("z", fc)], writes=[("x1b", fc, sb)])
                b.op("pool", lambda e: e.tensor_scalar(out=acc[:, fc, cs], in0=z[:, fc, :], scalar1=ALPHA,
                                                       scalar2=None, op0=ALU.mult),
                     reads=[("z", fc)], writes=[("acc", fc, sb)])

            _ln_feature_major(b, None, z, None, ones_f, ps_c, ps_m, lnt, ln1_sb, after_ln1, "ln1")

            for tc in range(SBK // 128):
                for kc in range(KC):
                    b.op("pe", lambda e, tc=tc, kc=kc: e.matmul(
                        ps_c[:, 0:NE], z[:, kc, tc * 128:(tc + 1) * 128], rw_sb[:, kc, :],
                        start=(kc == 0), stop=(kc == KC - 1)),
                        reads=[("z", kc), rw_sb.name], writes=[ps_c.name] if kc in (0, KC - 1) else [])
                b.op("dve", lambda e: e.tensor_tensor(out=lg[:, :], in0=ps_c[:, 0:NE], in1=rb_sb[:, :], op=ALU.add),
                     reads=[ps_c.name, rb_sb.name], writes=[lg.name])
                b.op("dve", lambda e: e.max(out=mx8[:, :], in_=lg[:, :]), reads=[lg.name], writes=[mx8.name])
                b.op("dve", lambda e: e.tensor_scalar(out=msk[:, :], in0=lg[:, :], scalar1=mx8[:, 3:4], scalar2=None,
                                                      op0=ALU.is_ge), reads=[lg.name, mx8.name], writes=[msk.name])
                b.op("dve", lambda e: e.tensor_scalar(out=nmx[:, :], in0=mx8[:, 0:1], scalar1=-1.0, scalar2=None,
                                                      op0=ALU.mult), reads=[mx8.name], writes=[nmx.name])
                b.op("act", lambda e: e.activation(out=ex[:, :], in_=lg[:, :], func=AF.Exp, bias=nmx[:, 0:1], scale=1.0),
                     reads=[lg.name, nmx.name], writes=[ex.name])
                b.op("dve", lambda e: e.tensor_tensor(out=ex[:, :], in0=ex[:, :], in1=msk[:, :], op=ALU.mult),
                     reads=[ex.name, msk.name], writes=[ex.name])
                b.op("dve", lambda e: e.reduce_sum(out=ssum[:, :], in_=ex[:, :], axis=AX.X),
                     reads=[ex.name], writes=[ssum.name])
                b.op("dve", lambda e: e.reciprocal(out=ssum[:, :], in_=ssum[:, :]), reads=[ssum.name], writes=[ssum.name])
                b.op("dve", lambda e: e.tensor_scalar(out=ex[:, :], in0=ex[:, :], scalar1=ssum[:, 0:1], scalar2=None,
                                                      op0=ALU.mult), reads=[ex.name, ssum.name], writes=[ex.name])
                b.op("pe", lambda e: e.transpose(out=ps_m[0:NE, 0:128], in_=ex[:, :], identity=id_sb[:, :]),
                     reads=[ex.name, id_sb.name], writes=[ps_m.name])
                cofs = sb * SBK + tc * 128
                b.op("act", lambda e, cofs=cofs: e.activation(out=combT[:, cofs:cofs + 128], in_=ps_m[0:NE, 0:128],
                                                              func=AF.Copy),
                     reads=[ps_m.name], writes=[("combT", sb, tc)])
            cs = slice(sb * SBK, (sb + 1) * SBK)
            for fo in range(KC):
                pp = ps_d[fo % 2]
                b.op("pe", lambda e, pp=pp, fo=fo, cs=cs: e.matmul(pp[:, :], bdn_sb[:, fo * 128:(fo + 1) * 128],
                                                                  combT[:, cs], start=True, stop=True),
                     reads=[bdn_sb.name] + [("combT", sb, tc) for tc in range(4)], writes=[pp.name])
                b.op("dve", lambda e, pp=pp, fo=fo, cs=cs: e.tensor_tensor(out=acc[:, fo, cs], in0=acc[:, fo, cs],
                                                                          in1=pp[:, :], op=ALU.add),
                     reads=[("acc", fo, sb), pp.name], writes=[("acc", fo, sb)])

        tile_q = []
        tcount = [0]

        def stage2(item):
            g_t, s_t, u_t, cb, ab, fc = item
            b.op("dve", lambda e: e.tensor_scalar(out=u_t[:, :], in0=u_t[:, :], scalar1=-6.0, scalar2=8.0,
                                                  op0=ALU.max, op1=ALU.min), reads=[u_t.name], writes=[u_t.name])
            b.op("dve", lambda e: e.tensor_tensor(out=u_t[:, :], in0=u_t[:, :], in1=cb[:, :], op=ALU.mult),
                 reads=[u_t.name, cb.name], writes=[u_t.name])
            b.op("dve", lambda e: e.tensor_tensor(out=g_t[:, :], in0=g_t[:, :], in1=s_t[:, :], op=ALU.mult),
                 reads=[g_t.name, s_t.name], writes=[g_t.name])
            b.op("dve", lambda e: e.tensor_tensor(out=ab[:, fc, :], in0=g_t[:, :], in1=u_t[:, :], op=ALU.mult),
                 reads=[g_t.name, u_t.name], writes=[(ab.name, fc)])

        def down(ex_d, fo):
            Dn, Dkey = next_unit()
            for sb in range(NSB):
                cs = slice(sb * SBK, (sb + 1) * SBK)
                ab = actp[ex_d % 2][sb]
                pd = ps_d[sb % 2]
                for fc in range(KC):
                    b.op("pe", lambda e, pd=pd, fc=fc, ab=ab, Dn=Dn: e.matmul(
                        pd[:, :], Dn[:, fc, :], ab[:, fc, :], start=(fc == 0), stop=(fc == KC - 1)),
                        reads=[Dkey, (ab.name, fc)], writes=[pd.name] if fc in (0, KC - 1) else [])
                b.op("dve", lambda e, pd=pd, fo=fo, cs=cs: e.tensor_tensor(out=acc[:, fo, cs], in0=acc[:, fo, cs],
                                                                          in1=pd[:, :], op=ALU.add),
                     reads=[("acc", fo, sb), pd.name], writes=[("acc", fo, sb)])
            unit_done()

        for ex_i in range(NE):
            par = ex_i % 2
            for sb in range(NSB):
                cs = slice(sb * SBK, (sb + 1) * SBK)
                cb = cbt[par][sb]
                b.op("pe", lambda e, cs=cs, ex_i=ex_i: e.matmul(ps_c[:, :], id_sb[0:NE, ex_i:ex_i + 1].broadcast_to([NE, 128]),
                                                               combT[:, cs], start=True, stop=True),
                     reads=[id_sb.name] + [("combT", sb, tc) for tc in range(4)], writes=[ps_c.name])
                b.op("act", lambda e, cb=cb: e.activation(out=cb[:, :], in_=ps_c[:, :], func=AF.Copy),
                     reads=[ps_c.name], writes=[cb.name])
            for fc in range(KC):
                G, Gkey = next_unit()
                U, Ukey = next_unit()
                for W, Wkey, pz in ((G, Gkey, ps_g), (U, Ukey, ps_u)):
                    for kc in range(KC):
                        for sb in range(NSB):
                            cs = slice(sb * SBK, (sb + 1) * SBK)
                            b.op("pe", lambda e, W=W, pz=pz, kc=kc, sb=sb, cs=cs: e.matmul(
                                pz[sb][:, :], W[:, kc, :], x1b[:, kc, cs], start=(kc == 0), stop=(kc == KC - 1)),
                                reads=[Wkey, ("x1b", kc, sb)], writes=[pz[sb].name] if kc in (0, KC - 1) else [])
                unit_done()
                bg = bgu_sb[:, ex_i * 16 + fc: ex_i * 16 + fc + 1]
                bu = bgu_sb[:, ex_i * 16 + 8 + fc: ex_i * 16 + 8 + fc + 1]
                for sb in range(NSB):
                    ti = tcount[0] % 2
                    tcount[0] += 1
                    g_t = tg[ti]; s_t = ts[ti]; u_t = tu[ti]
                    pg = ps_g[sb]; pu = ps_u[sb]
                    b.op("dve", lambda e, pg=pg, g_t=g_t, bg=bg: e.tensor_scalar(
                        out=g_t[:, :], in0=pg[:, :], scalar1=bg, scalar2=7.0, op0=ALU.add, op1=ALU.min),
                        reads=[pg.name, bgu_sb.name], writes=[g_t.name])
                    b.op("act", lambda e, g_t=g_t, s_t=s_t: e.activation(out=s_t[:, :], in_=g_t[:, :],
                                                                         func=AF.Sigmoid, scale=1.702),
                         reads=[g_t.name], writes=[s_t.name])
                    b.op("act", lambda e, pu=pu, u_t=u_t, bu=bu: e.activation(out=u_t[:, :], in_=pu[:, :],
                                                                             func=AF.Identity, bias=bu, scale=1.0),
                         reads=[pu.name, bgu_sb.name], writes=[u_t.name])
                    if tile_q:
                        stage2(tile_q.pop(0))
                    tile_q.append((g_t, s_t, u_t, cbt[par][sb], actp[par][sb], fc))
                if ex_i >= 1:
                    down(ex_i - 1, fc)
        while tile_q:
            stage2(tile_q.pop(0))
        for fo in range(KC):
            down(NE - 1, fo)

        for sb in range(NSB):
            cs = slice(sb * SBK, (sb + 1) * SBK)
            c0 = t0 + sb * SBK
            for fc in range(KC):
                b.op("act", lambda e, fc=fc, cs=cs: e.activation(out=z[:, fc, :], in_=acc[:, fc, cs], func=AF.Copy),
                     reads=[("acc", fc, sb)], writes=[("z", fc)])

            def after_ln2(fc, c0=c0):
                o = b.dma("sp", "yout", lambda e: e.dma_start(out=y_f32[fc * 128:(fc + 1) * 128, c0:c0 + SBK],
                                                              in_=z[:, fc, :]), reads=[("z", fc)])
                out_ops.append(o)
                if y_b16 is not None:
                    zb_ = zb[fc % 2]
                    b.op("act", lambda e: e.activation(out=zb_[:, :], in_=z[:, fc, :], func=AF.Copy),
                         reads=[("z", fc)], writes=[zb_.name])
                    o2 = b.dma("sp", "youtb%d" % (fc % 2), lambda e: e.dma_start(
                        out=y_b16[fc // 2, (fc % 2) * 128:(fc % 2 + 1) * 128, c0:c0 + SBK], in_=zb_[:, :]),
                        reads=[zb_.name])
                    out_ops.append(o2)

            _ln_feature_major(b, None, z, None, ones_f, ps_c, ps_m, lnt, ln2_sb, after_ln2, "ln2")

    b.emit(final_waits=out_ops)
    b.close()


def attn_phase(nc, semstack, tag, io, S=8192):
    NQB = None
    NTB = S // SBK
    NKB = S // 128
    NRP = S // 128
    if NQB is None:
        NQB = NTB

    w_sel = io["w_sel"]; t5c = io["t5c"]; t5b = io["t5b"]; lamv = io["lamv"]; cst = io["cst"]
    gsub = io["gsub"]; nab = io["nab"]; o_scr = io["o_scr"]; x_src = io["x_src"]

    b = Builder(nc, semstack, tag)
    BQ = [b.sbuf(f"BQ{i}", [128, S], BF16) for i in range(2)]
    BK = [b.sbuf(f"BK{i}", [128, S], BF16) for i in range(2)]
    BV = b.sbuf("BV", [128, NKB, 256], BF16)
    wsb = b.sbuf("wsb", [128, KC, 768], BF16)
    xb = [b.sbuf(f"xb{i}", [128, KC, SBK], BF16) for i in range(2)]
    t5b_sb = b.sbuf("t5b_sb", [128, 12, SBK], F32)
    nab_sb = [b.sbuf(f"nab{i}", [128, 640], F32) for i in range(2)]
    t5c_sb = b.sbuf("t5c_sb", [128, 4], F32)
    lam_sb = b.sbuf("lam_sb", [128, 256], F32); cst_sb = b.sbuf("cst_sb", [128, 4], F32)
    gcol = b.sbuf("gcol", [128, 1], F32); nlam = b.sbuf("nlam", [128, 1], F32)
    e12 = b.sbuf("e12", [128, 2], F32); lprod = b.sbuf("lprod", [128, 128], F32)
    ones_b = b.sbuf("ones_b", [128, 128], BF16); ones_f = b.sbuf("ones_f", [128, 128], F32)
    tA = b.sbuf("tA", [128, SBK], F32); tB = b.sbuf("tB", [128, SBK], F32)
    tAo = [b.sbuf(f"tAo{i}", [128, SBK], BF16) for i in range(2)]
    nsb = [b.sbuf(f"nsb{i}", [128, 640], F32) for i in range(2)]
    nP = [b.sbuf(f"nP{i}", [128, 640], BF16) for i in range(2)]
    nr = b.sbuf("nr", [64, 128], F32)
    nout = [b.sbuf(f"nout{i}", [64, SBK], BF16) for i in range(2)]

    class _Bank:
        def __init__(self, t, j):
            self.t = t; self.j = j; self.name = t.name + "_b%d" % j

        def __getitem__(self, idx):
            return self.t[:, self.j, :][idx]

    PP = [b.psum(f"PP{i}", [128, 2, SBK]) for i in range(4)]
    psb = [_Bank(PP[i // 2], i % 2) for i in range(8)]
    Pt2 = [b.sbuf(f"Pt2_{i}", [128, 2, SBK], BF16) for i in range(2)]
    tsp2 = b.sbuf("tsp2", [128, 2, SBK], F32)
    dacc = b.sbuf("dacc", [128, 2, SBK], F32)

    def ld(eng, slot, dst, src, key):
        b.dma(eng, slot, lambda e: e.dma_start(out=dst, in_=src), writes=[key])

    ld("sp", "c0", t5c_sb[:, :], t5c[:, :], t5c_sb.name)
    ld("sp", "c0", lam_sb[:, :], lamv[0:1, :].broadcast_to([128, 256]), lam_sb.name)
    ld("sp", "c0", cst_sb[:, :], cst[:, :], cst_sb.name)
    ld("sp", "c0", gcol[:, :], gsub[:, :], gcol.name)
    for h in range(2):
        for v in range(6):
            ld("sp", "c0", t5b_sb[:, h * 6 + v, :], t5b[h, v, :, :], t5b_sb.name)
    b.op("dve", lambda e: e.memset(ones_f[:, :], 1.0), writes=[ones_f.name])
    b.op("dve", lambda e: e.memset(ones_b[:, :], 1.0), writes=[ones_b.name])
    b.op("dve", lambda e: e.tensor_tensor(out=lprod[:, 0:64], in0=lam_sb[:, 0:64], in1=lam_sb[:, 64:128], op=ALU.mult),
         reads=[lam_sb.name], writes=[lprod.name])
    b.op("dve", lambda e: e.tensor_tensor(out=lprod[:, 64:128], in0=lam_sb[:, 128:192], in1=lam_sb[:, 192:256], op=ALU.mult),
         reads=[lam_sb.name, lprod.name], writes=[lprod.name])
    b.op("dve", lambda e: e.reduce_sum(out=e12[:, 0:1], in_=lprod[:, 0:64], axis=AX.X), reads=[lprod.name], writes=[e12.name])
    b.op("dve", lambda e: e.reduce_sum(out=e12[:, 1:2], in_=lprod[:, 64:128], axis=AX.X), reads=[lprod.name, e12.name], writes=[e12.name])
    b.op("act", lambda e: e.activation(out=e12[:, :], in_=e12[:, :], func=AF.Exp), reads=[e12.name], writes=[e12.name])
    b.op("dve", lambda e: e.tensor_tensor(out=nlam[:, :], in0=e12[:, 1:2], in1=e12[:, 0:1], op=ALU.subtract),
         reads=[e12.name], writes=[nlam.name])
    b.op("dve", lambda e: e.tensor_tensor(out=nlam[:, :], in0=nlam[:, :], in1=cst_sb[:, 0:1], op=ALU.subtract),
         reads=[nlam.name, cst_sb.name], writes=[nlam.name])
    b.op("dve", lambda e: e.tensor_tensor(out=gcol[:, :], in0=gcol[:, :], in1=cst_sb[:, 1:2], op=ALU.mult),
         reads=[gcol.name, cst_sb.name], writes=[gcol.name])

    out_ops = []

    def in_proj(col0):
        b.dma("pool", "wsb", lambda e: e.dma_start(
            out=wsb[:, :, :], in_=w_sel[:, col0:col0 + 768].rearrange("(kc p) f -> p kc f", p=128)),
            writes=[wsb.name])
        for tb in range(NTB):
            x_ = xb[tb % 2]
            if io["x_3d"]:
                b.dma("pool", "xb%d" % (tb % 2), lambda e, x_=x_, tb=tb: e.dma_start(
                    out=x_[:, :, :], in_=x_src(tb)), writes=[x_.name])
            else:
                for j in range(2):
                    b.dma("pool", "xb%d" % (tb % 2), lambda e, x_=x_, tb=tb, j=j: e.dma_start(
                        out=x_[:, :, :].rearrange("p (i j) t -> p i j t", i=4)[:, :, j, :],
                        in_=x_src(tb, j)), writes=[x_.name])
            for oc in range(4):
                pp = psb[oc % 2]
                for kc in range(KC):
                    b.op("pe", lambda e, pp=pp, kc=kc, oc=oc, x_=x_: e.matmul(
                        pp[:, :], wsb[:, kc, oc * 128:(oc + 1) * 128], x_[:, kc, :],
                        start=(kc == 0), stop=(kc == KC - 1)),
                        reads=[wsb.name, x_.name], writes=[pp.name] if kc in (0, KC - 1) else [])
                dst = (BQ if oc < 2 else BK)[oc % 2]
                sc = 0.125 if oc < 2 else 1.0
                eng = "act" if oc % 2 == 0 else "dve"
                if eng == "act":
                    b.op("act", lambda e, pp=pp, dst=dst, tb=tb, sc=sc: e.activation(
                        out=dst[:, tb * SBK:(tb + 1) * SBK], in_=pp[:, :], func=AF.Copy, scale=sc),
                        reads=[pp.name], writes=[(dst.name, tb)])
                else:
                    b.op("dve", lambda e, pp=pp, dst=dst, tb=tb, sc=sc: e.tensor_scalar(
                        out=dst[:, tb * SBK:(tb + 1) * SBK], in0=pp[:, :], scalar1=sc, scalar2=None, op0=ALU.mult),
                        reads=[pp.name], writes=[(dst.name, tb)])
            for tc in range(4):
                pp = psb[2 + tc % 2]
                for kc in range(KC):
                    b.op("pe", lambda e, pp=pp, kc=kc, tc=tc, x_=x_: e.matmul(
                        pp[:, 0:256], x_[:, kc, tc * 128:(tc + 1) * 128], wsb[:, kc, 512:768],
                        start=(kc == 0), stop=(kc == KC - 1)),
                        reads=[wsb.name, x_.name], writes=[pp.name] if kc in (0, KC - 1) else [])
                ch = tb * 4 + tc
                if tc % 2 == 0:
                    b.op("act", lambda e, pp=pp, ch=ch: e.activation(out=BV[:, ch, :], in_=pp[:, 0:256], func=AF.Copy),
                         reads=[pp.name], writes=[("BV", ch)])
                else:
                    b.op("dve", lambda e, pp=pp, ch=ch: e.tensor_copy(out=BV[:, ch, :], in_=pp[:, 0:256]),
                         reads=[pp.name], writes=[("BV", ch)])

    in_proj(0)
    for h in range(2):
        for qb in range(NQB):
            qs = slice(qb * SBK, (qb + 1) * SBK)
            po = [psb[4], psb[5]]; pd = [psb[6], psb[7]]

            def qk(kb, h=h, qb=qb, qs=qs):
                for m in range(2):
                    pp = psb[(kb % 2) * 2 + m]
                    b.op("pe", lambda e, pp=pp, m=m: e.matmul(
                        pp[:, :], BK[h][m * 64:(m + 1) * 64, kb * 128:(kb + 1) * 128], BQ[h][m * 64:(m + 1) * 64, qs],
                        start=True, stop=True),
                        reads=[(BK[h].name, kb // 4), (BQ[h].name, qb)], writes=[pp.name])

            def expav(kb, h=h, qb=qb):
                rel = kb - 4 * qb
                PPk = PP[kb % 2]
                pk = [psb[(kb % 2) * 2].name, psb[(kb % 2) * 2 + 1].name]
                P = Pt2[kb % 2]
                if -1 <= rel <= 4:
                    bt = t5b_sb[:, h * 6 + rel + 1, :]
                    for m in range(2):
                        b.op("dve", lambda e, m=m, bt=bt, PPk=PPk: e.tensor_tensor(
                            out=tsp2[:, m, :], in0=PPk[:, m, :], in1=bt, op=ALU.add),
                            reads=[pk[m], t5b_sb.name], writes=[(tsp2.name, m)])
                    b.op("act", lambda e, P=P: e.activation(out=P[:, :, :], in_=tsp2[:, :, :], func=AF.Exp),
                         reads=[(tsp2.name, 0), (tsp2.name, 1)], writes=[P.name])
                else:
                    col = h * 2 + (0 if rel < 0 else 1)
                    b.op("act", lambda e, PPk=PPk, P=P, col=col: e.activation(
                        out=P[:, :, :], in_=PPk[:, :, :], func=AF.Exp, bias=t5c_sb[:, col:col + 1], scale=1.0),
                        reads=pk + [t5c_sb.name], writes=[P.name])
                for m in range(2):
                    b.op("pe", lambda e, P=P, m=m: e.matmul(
                        po[m][:, :], BV[:, kb, h * 128:(h + 1) * 128], P[:, m, :], start=(kb == 0), stop=(kb == NKB - 1)),
                        reads=[("BV", kb), P.name], writes=[po[m].name] if kb in (0, NKB - 1) else [])
                if kb == 0:
                    b.op("dve", lambda e, P=P: e.tensor_copy(out=dacc[:, :, :], in_=P[:, :, :]),
                         reads=[P.name], writes=[dacc.name])
                else:
                    b.op("dve", lambda e, P=P: e.tensor_tensor(out=dacc[:, :, :], in0=dacc[:, :, :], in1=P[:, :, :], op=ALU.add),
                         reads=[P.name, dacc.name], writes=[dacc.name])

            qk(0)
            for kb in range(NKB):
                if kb + 1 < NKB:
                    qk(kb + 1)
                expav(kb)
            for m in range(2):
                b.op("pe", lambda e, m=m: e.matmul(pd[m][:, :], ones_f[:, :], dacc[:, m, :], start=True, stop=True),
                     reads=[ones_f.name, dacc.name], writes=[pd[m].name])
            b.op("dve", lambda e: e.reciprocal(out=tA[:, :], in_=pd[0][:, :]), reads=[pd[0].name], writes=[tA.name])
            b.op("dve", lambda e: e.tensor_tensor(out=tA[:, :], in0=po[0][:, :], in1=tA[:, :], op=ALU.mult),
                 reads=[po[0].name, tA.name], writes=[tA.name])
            b.op("dve", lambda e: e.reciprocal(out=tB[:, :], in_=pd[1][:, :]), reads=[pd[1].name], writes=[tB.name])
            b.op("dve", lambda e: e.tensor_tensor(out=tB[:, :], in0=po[1][:, :], in1=tB[:, :], op=ALU.mult),
                 reads=[po[1].name, tB.name], writes=[tB.name])
            b.op("dve", lambda e: e.scalar_tensor_tensor(out=tA[:, :], in0=tB[:, :], scalar=nlam[:, 0:1], in1=tA[:, :],
                                                         op0=ALU.mult, op1=ALU.add),
                 reads=[tA.name, tB.name, nlam.name], writes=[tA.name])
            b.op("act", lambda e: e.activation(out=tB[:, :], in_=tA[:, :], func=AF.Square), reads=[tA.name], writes=[tB.name])
            pn = psb[0]
            b.op("pe", lambda e: e.matmul(pn[:, :], ones_f[:, :], tB[:, :], start=True, stop=True),
                 reads=[ones_f.name, tB.name], writes=[pn.name])
            b.op("dve", lambda e: e.tensor_scalar(out=tB[:, :], in0=pn[:, :], scalar1=1.0 / 128, scalar2=1e-5,
                                                  op0=ALU.mult, op1=ALU.add), reads=[pn.name], writes=[tB.name])
            b.op("act", lambda e: e.activation(out=tB[:, :], in_=tB[:, :], func=AF.Sqrt), reads=[tB.name], writes=[tB.name])
            b.op("dve", lambda e: e.reciprocal(out=tB[:, :], in_=tB[:, :]), reads=[tB.name], writes=[tB.name])
            b.op("dve", lambda e: e.tensor_tensor(out=tA[:, :], in0=tA[:, :], in1=tB[:, :], op=ALU.mult),
                 reads=[tA.name, tB.name], writes=[tA.name])
            to_ = tAo[qb % 2]
            b.op("dve", lambda e, to_=to_: e.tensor_scalar(out=to_[:, :], in0=tA[:, :], scalar1=gcol[:, 0:1], scalar2=None, op0=ALU.mult),
                 reads=[tA.name, gcol.name], writes=[to_.name])
            o = b.dma("sp", "oda%d" % (qb % 2), lambda e, h=h, qs=qs, to_=to_: e.dma_start(out=o_scr[h, :, qs], in_=to_[:, :]),
                      reads=[to_.name])
            out_ops.append(o)

    in_proj(768)
    NRPQ = NRP if NQB == NTB else NQB * 4
    na_units = []
    for n in range(4):
        for rp in range(NRPQ):
            na_units.append((n, rp))

    def na_qk(ui):
        n, rp = na_units[ui]
        p = n // 2
        pb = (n % 2) * 64
        cs = min(max(rp - 2, 0), NRP - 5)
        if rp == 0:
            v = 1
        elif rp == 1:
            v = 2
        elif rp == NRP - 2:
            v = 3
        elif rp == NRP - 1:
            v = 4
        else:
            v = 0
        i2 = ui % 2
        nb_ = nab_sb[i2]
        b.dma("sp", "nab%d" % i2, lambda e, nb_=nb_, n=n, v=v: e.dma_start(out=nb_[:, :], in_=nab[n, v, :, :]),
              writes=[nb_.name])
        psA = psb[i2 * 2]; psB = psb[i2 * 2 + 1]
        for c in range(5):
            dst = psA[:, c * 128:(c + 1) * 128] if c < 4 else psB[:, 0:128]
            b.op("pe", lambda e, dst=dst, c=c, p=p, pb=pb, cs=cs, rp=rp: e.matmul(
                dst, BK[p][pb:pb + 64, (cs + c) * 128:(cs + c + 1) * 128], BQ[p][pb:pb + 64, rp * 128:(rp + 1) * 128],
                start=True, stop=True),
                reads=[(BK[p].name, (cs + c) // 4), (BQ[p].name, rp // 4)],
                writes=[psA.name if c < 4 else psB.name])

    def na_rest(ui):
        n, rp = na_units[ui]
        cs = min(max(rp - 2, 0), NRP - 5)
        i2 = ui % 2
        nb_ = nab_sb[i2]
        psA = psb[i2 * 2]; psB = psb[i2 * 2 + 1]
        ppo = psb[4 + i2]; ppd = psb[6 + i2]
        sb_ = nsb[i2]; P = nP[i2]
        b.op("dve", lambda e: e.tensor_tensor(out=sb_[:, 0:512], in0=psA[:, :], in1=nb_[:, 0:512], op=ALU.add),
             reads=[psA.name, nb_.name], writes=[(sb_.name, 0)])
        b.op("dve", lambda e: e.tensor_tensor(out=sb_[:, 512:640], in0=psB[:, 0:128], in1=nb_[:, 512:640], op=ALU.add),
             reads=[psB.name, nb_.name], writes=[(sb_.name, 1)])
        b.op("act", lambda e: e.activation(out=P[:, :], in_=sb_[:, :], func=AF.Exp),
             reads=[(sb_.name, 0), (sb_.name, 1)], writes=[P.name])
        for c in range(5):
            b.op("pe", lambda e, c=c: e.matmul(
                ppo[0:64, 0:128], BV[:, cs + c, n * 64:(n + 1) * 64], P[:, c * 128:(c + 1) * 128],
                start=(c == 0), stop=(c == 4)),
                reads=[("BV", cs + c), P.name], writes=[ppo.name] if c in (0, 4) else [])
        for c in range(5):
            b.op("pe", lambda e, c=c: e.matmul(
                ppd[0:64, 0:128], ones_b[:, 0:64], P[:, c * 128:(c + 1) * 128], start=(c == 0), stop=(c == 4)),
                reads=[ones_b.name, P.name], writes=[ppd.name] if c in (0, 4) else [])
        no = nout[(rp // 4) % 2]
        b.op("dve", lambda e: e.reciprocal(out=nr[:, :], in_=ppd[0:64, 0:128]), reads=[ppd.name], writes=[nr.name])
        b.op("dve", lambda e: e.tensor_tensor(
            out=no[:, (rp % 4) * 128:(rp % 4 + 1) * 128], in0=ppo[0:64, 0:128], in1=nr[:, :], op=ALU.mult),
            reads=[ppo.name, nr.name], writes=[(no.name, rp % 4)])
        if rp % 4 == 3:
            r0 = (rp // 4) * SBK
            o = b.dma("sp", "ona%d" % ((rp // 4) % 2), lambda e: e.dma_start(
                out=o_scr[2 + n // 2, (n % 2) * 64:(n % 2 + 1) * 64, r0:r0 + SBK], in_=no[:, :]),
                reads=[(no.name, i) for i in range(4)])
            out_ops.append(o)

    na_qk(0)
    for ui in range(len(na_units)):
        if ui + 1 < len(na_units):
            na_qk(ui + 1)
        na_rest(ui)

    b.emit(final_waits=out_ops)
    b.close()


import math as _math

HEAD_DIM = 64
GRID_W = 64
NEG = -30000.0


def _t5_bucket_np(rel):
    rel = np.asarray(rel, np.int32)
    half = 16
    max_exact = 8
    ret = (rel > 0).astype(np.int32) * half
    n = np.abs(rel)
    n_f = np.maximum(n, 1).astype(np.float32)
    large = max_exact + (np.log(n_f / np.float32(max_exact)) / np.float32(_math.log(128 / max_exact))
                         * np.float32(half - max_exact)).astype(np.int32)
    large = np.minimum(large, half - 1)
    return ret + np.where(n < max_exact, n, large)


def _t5_tiles_idx():
    p = np.arange(128)[:, None]
    j = np.arange(SBK)[None, :]
    return np.stack([_t5_bucket_np((v - 1) * 128 + p - j) for v in range(6)])


def _na_tiles_idx(rows):
    nrp = rows // 2
    reps = {0: 2 if nrp > 4 else None, 1: 0, 2: 1, 3: nrp - 2, 4: nrp - 1}
    col_start = np.clip(np.arange(GRID_W) - 8, 0, GRID_W - 16)
    ro = np.zeros((5, 128, 640), np.int64); co = np.zeros((5, 128, 640), np.int64)
    va = np.zeros((5, 128, 640), bool)
    kic = np.arange(128)
    q = np.arange(128)
    for v, rp in reps.items():
        if rp is None:
            continue
        cs = min(max(rp - 2, 0), nrp - 5)
        for c in range(5):
            krow = 2 * (cs + c) + kic // 64
            kcol = kic % 64
            r = 2 * rp + q // 64
            w = q % 64
            rs = np.clip(r - 4, 0, rows - 8)
            valid = ((krow[:, None] >= rs[None, :]) & (krow[:, None] < rs[None, :] + 8) &
                     (kcol[:, None] >= col_start[w][None, :]) & (kcol[:, None] < col_start[w][None, :] + 16))
            roff = krow[:, None] - r[None, :] + 7
            coff = kcol[:, None] - w[None, :] + 15
            sl = slice(c * 128, (c + 1) * 128)
            va[v, :, sl] = valid
            ro[v, :, sl] = np.where(valid, roff, 0)
            co[v, :, sl] = np.where(valid, coff, 0)
    return ro, co, va


def prep_attn_inputs(x_b, w_in_l, hh, layer, lq1, lk1, lq2, lk2, subln_g, t5_table, na_rpb_l, S=None):
    f = np.float32
    S = x_b.shape[0] if S is None else S
    dah = [2 * hh, 2 * hh + 1]
    nah = [4 * hh + i for i in range(4)]
    o_q1, o_q2, o_k1, o_k2, o_va, o_qn, o_kn, o_vn = 0, 256, 512, 768, 1024, 1536, 2048, 2560
    cols = []
    for h in dah:
        cols += list(range(o_q1 + 64 * h, o_q1 + 64 * h + 64)) + list(range(o_q2 + 64 * h, o_q2 + 64 * h + 64))
    for h in dah:
        cols += list(range(o_k1 + 64 * h, o_k1 + 64 * h + 64)) + list(range(o_k2 + 64 * h, o_k2 + 64 * h + 64))
    for h in dah:
        cols += list(range(o_va + 128 * h, o_va + 128 * h + 128))
    for base in (o_qn, o_kn, o_vn):
        for n in nah:
            cols += list(range(base + 64 * n, base + 64 * n + 64))
    w_sel = np.ascontiguousarray(w_in_l[:, cols]).astype(f)
    t5c = np.empty((128, 4), f)
    for i, h in enumerate(dah):
        t5c[:, 2 * i] = t5_table[15, h]
        t5c[:, 2 * i + 1] = t5_table[31, h]
    tidx = _t5_tiles_idx()
    t5b = np.stack([t5_table[:, h][tidx] for h in dah]).astype(f)
    lamv = np.concatenate([lq1, lk1, lq2, lk2]).astype(f)[None, :]
    lam_init = 0.8 - 0.6 * _math.exp(-0.3 * layer)
    cst = np.zeros((128, 4), f); cst[:, 0] = lam_init; cst[:, 1] = 1.0 - lam_init
    ro, co, va = _na_tiles_idx(S // GRID_W)
    nab = np.stack([np.where(va, na_rpb_l[n][ro, co], f(NEG)) for n in nah]).astype(f)
    return dict(w_sel=w_sel, t5c=t5c, t5b=t5b, lamv=lamv, cst=cst,
                gsub=np.ascontiguousarray(subln_g.astype(f)[:, None]), nab=nab)


def _pack_gb(g, bb):
    return np.ascontiguousarray(np.concatenate([g.reshape(8, 128).T, bb.reshape(8, 128).T], axis=1)).astype(np.float32)


PAIRS = [[0, 1], [2, 3], [4, 5], [6, 7]]


def xchg_phase(nc, semstack, tag, pairs):
    sem = semstack.enter_context(nc.semaphore("cc_" + tag))
    with nc.Block() as block:
        @block.gpsimd
        def _(g):
            for src, dst in pairs:
                g.collective_compute("AllGather", ALU.bypass, replica_groups=PAIRS,
                                     ins=[src], outs=[dst]).then_inc(sem)
            g.wait_ge(sem, len(pairs))


def build_fused(S=8192, NE=32, depth=2, TB=1024):
    nc = bass.Bass("TRN2", target_bir_lowering=False)
    T = S // 2

    def din(name, shape):
        return nc.dram_tensor(name, list(shape), F32, kind="ExternalInput").ap()

    xT_full = din("xT_full", [D, S]); xT_own = din("xT_own", [D, T]); smask = din("smask", [128, 2])
    w_sel = din("w_sel", [depth, D, 1536]); t5c = din("t5c", [128, 4]); t5b = din("t5b", [2, 6, 128, SBK])
    lamv = din("lamv", [depth, 256]); cst = din("cst", [depth, 128, 4]); gsub = din("gsub", [depth, 128, 1])
    nab = din("nab", [depth, 4, 5, 128, 640])
    w_out = din("w_out", [depth, D, D]); ln1 = din("ln1", [depth, 128, 16]); ln2 = din("ln2", [depth, 128, 16])
    rw = din("rw", [depth, D, NE]); rb = din("rb", [depth, NE])
    wgu = din("wgu", [depth, NE, 16, 128, 1024]); bguT = din("bguT", [depth, 128, NE * 16])
    wdn = din("wdn", [depth, NE, 8, 128, 1024]); bdn = din("bdn", [depth, NE, D])
    ident = din("ident", [128, 128])
    yT = nc.dram_tensor("yT", [D, T], F32, kind="ExternalOutput").ap()
    o_scr = nc.dram_tensor("o_scr", [4, 128, S], BF16).ap()
    og = nc.dram_tensor("og", [4, 2, 128, S], BF16).ap()
    x1_scr = nc.dram_tensor("x1_scr", [D, T], F32).ap()
    xb_scr = nc.dram_tensor("xb_scr", [4, 256, T], BF16).ap()
    xg = nc.dram_tensor("xg", [4, 2, 2, 128, T], BF16).ap()

    semstack = contextlib.ExitStack()
    nblk = T // SBK
    for l in range(depth):
        if l == 0:
            x_src = lambda tb: xT_full[:, tb * SBK:(tb + 1) * SBK].rearrange("(kc p) t -> p kc t", p=128)
        else:
            x_src = lambda tb, j: xg[:, tb // nblk, j, :, (tb % nblk) * SBK:(tb % nblk + 1) * SBK].rearrange(
                "i p t -> p i t")
        attn_phase(nc, semstack, "a%d_" % l, dict(
            w_sel=w_sel[l], t5c=t5c, t5b=t5b, lamv=lamv[l:l + 1, :], cst=cst[l], gsub=gsub[l], nab=nab[l],
            o_scr=o_scr, x_src=x_src, x_3d=(l == 0)), S=S)
        xchg_phase(nc, semstack, "xo%d" % l,
                   [(o_scr[i], og[i].rearrange("r p t -> (r p) t")) for i in range(4)])
        last = (l == depth - 1)
        moe_phase(nc, semstack, "m%d_" % l, dict(
            og=og, xres=(xT_own if l == 0 else x1_scr), w_out=w_out[l], ln1=ln1[l], ln2=ln2[l], rw=rw[l],
            rb=rb[l:l + 1, :], wgu=wgu[l], bguT=bguT[l], wdn=wdn[l], bdn=bdn[l], ident=ident, smask=smask,
            y_f32=(yT if last else x1_scr), y_b16=(None if last else xb_scr)), T=T, NE=NE, TB=TB)
        if not last:
            xchg_phase(nc, semstack, "xx%d" % l,
                       [(xb_scr[i], xg[i].rearrange("r j p t -> (r j p) t")) for i in range(4)])
    semstack.close()
    return nc


def _relayout_gu(w):
    L, E = w.shape[0], w.shape[1]
    v = w.reshape(L, E, 8, 128, 16, 128)
    order = [j for fc in range(8) for j in (fc, 8 + fc)]
    out = np.empty((L, E, 16, 128, 8, 128), np.float32)
    for ui, j in enumerate(order):
        out[:, :, ui] = v[:, :, :, :, j, :].transpose(0, 1, 3, 2, 4)
    return out.reshape(L, E, 16, 128, 1024)


def _relayout_dn(w):
    L, E = w.shape[0], w.shape[1]
    v = w.reshape(L, E, 8, 128, 8, 128)
    return np.ascontiguousarray(v.transpose(0, 1, 4, 3, 2, 5)).reshape(L, E, 8, 128, 1024)


def _wout_perm():
    rows = []
    for i in range(4):
        for r in range(2):
            st = [(2 * r) * 128, (2 * r + 1) * 128, 512 + (4 * r) * 64, 512 + (4 * r + 2) * 64][i]
            rows += list(range(st, st + 128))
    return rows


def make_in_maps(x, w_in, w_out, lambda_q1, lambda_k1, lambda_q2, lambda_k2, subln_g, t5_table, na_rpb,
                 ln1_g, ln1_b, router_w, router_b, w_gate_up, b_gate_up, w_down, b_down, ln2_g, ln2_b):
    f = np.float32
    A = lambda a: np.asarray(a, dtype=f)
    x = A(x)
    B_, S_, _ = x.shape
    depth = w_in.shape[0]
    NE = router_w.shape[-1]
    T = S_ // 2
    w_in = A(w_in)
    perm = _wout_perm()
    common = dict(
        w_out=np.ascontiguousarray(A(w_out)[:, perm, :]),
        ln1=np.stack([_pack_gb(A(ln1_g)[l], A(ln1_b)[l]) for l in range(depth)]),
        ln2=np.stack([_pack_gb(A(ln2_g)[l], A(ln2_b)[l]) for l in range(depth)]),
        rw=A(router_w), rb=A(router_b), wgu=_relayout_gu(A(w_gate_up)),
        bguT=np.stack([np.ascontiguousarray(A(b_gate_up)[l].reshape(NE, 16, 128).transpose(2, 0, 1).reshape(128, NE * 16))
                       for l in range(depth)]),
        wdn=_relayout_dn(A(w_down)), bdn=A(b_down), ident=np.eye(128, dtype=f))
    in_maps = []
    xTs = [np.ascontiguousarray(x[bi].T) for bi in range(B_)]
    for c in range(2 * B_):
        bi, r = c // 2, c % 2
        per = [prep_attn_inputs(x[bi][:8], w_in[l], r, l, A(lambda_q1)[l], A(lambda_k1)[l], A(lambda_q2)[l],
                                A(lambda_k2)[l], A(subln_g)[l], A(t5_table), A(na_rpb)[l], S=S_) for l in range(depth)]
        m = dict(common)
        m["xT_full"] = xTs[bi]
        m["xT_own"] = np.ascontiguousarray(xTs[bi][:, r * T:(r + 1) * T])
        sm = np.zeros((128, 2), f); sm[:, r] = 1.0
        m["smask"] = sm
        m["w_sel"] = np.stack([p["w_sel"] for p in per])
        m["t5c"] = per[0]["t5c"]; m["t5b"] = per[0]["t5b"]
        m["lamv"] = np.concatenate([p["lamv"] for p in per], axis=0)
        m["cst"] = np.stack([p["cst"] for p in per]); m["gsub"] = np.stack([p["gsub"] for p in per])
        m["nab"] = np.stack([p["nab"] for p in per])
        in_maps.append(m)
    return in_maps, (B_, S_, T)


def kernel(**inputs):
    in_maps, (B_, S_, T) = make_in_maps(**inputs)
    NE = np.asarray(inputs["router_w"]).shape[-1]
    depth = np.asarray(inputs["w_in"]).shape[0]
    nc = build_fused(S=S_, NE=NE, depth=depth)
    res = run_bass_kernel_spmd(nc, in_maps, core_ids=list(range(2 * B_)))
    out = np.empty((B_, S_, D), np.float32)
    for c in range(2 * B_):
        bi, r = c // 2, c % 2
        out[bi, r * T:(r + 1) * T, :] = res.results[c]["yT"].T
    return out
```

```python
import contextlib
import numpy as np
import concourse.bass as bass
import concourse.mybir as mybir
from concourse.bass_utils import run_bass_kernel_spmd

F32 = mybir.dt.float32
BF16 = mybir.dt.bfloat16
AF = mybir.ActivationFunctionType
ALU = mybir.AluOpType
AX = mybir.AxisListType

ENGS = ("pe", "act", "dve", "pool", "sp")


class Op:
    __slots__ = ("eng", "fn", "deps", "needed", "val", "ctr", "step")

    def __init__(self, eng, fn, ctr, step):
        self.eng = eng
        self.fn = fn
        self.deps = []
        self.needed = False
        self.val = 0
        self.ctr = ctr
        self.step = step


class Builder:
    def __init__(self, nc, semstack=None, tag=""):
        self.nc = nc
        self.semstack = semstack
        self.tag = tag
        self.ops = {e: [] for e in ENGS}
        self.lastw = {}
        self.readers = {}
        self.ctr_ops = {}
        self.stack = contextlib.ExitStack()
        self.dma_slots = set()

    def sbuf(self, name, shape, dt):
        return self.stack.enter_context(self.nc.sbuf_tensor(self.tag + name, list(shape), dt))

    def psum(self, name, shape, dt=F32):
        return self.stack.enter_context(self.nc.psum_tensor(self.tag + name, list(shape), dt))

    def _record(self, op, reads, writes):
        e = op.eng
        deps = []
        for k in reads:
            w = self.lastw.get(k)
            if w is not None:
                deps.append(w)
        for k in writes:
            w = self.lastw.get(k)
            if w is not None:
                deps.append(w)
            for r in self.readers.get(k, ()):
                if r.eng == e and r.step == 1 and op.step == 1:
                    continue
                deps.append(r)
        for d in deps:
            if d is op:
                continue
            if e == "pe" and d.eng == "pe" and d.step == 1 and op.step == 1:
                continue
            d.needed = True
            if d.step == 16:
                op.deps.append((d.ctr, 16 * len(self.ctr_ops[d.ctr])))
            else:
                op.deps.append(d)
        for k in reads:
            self.readers.setdefault(k, []).append(op)
        for k in writes:
            self.lastw[k] = op
            self.readers[k] = []
        self.ops[e].append(op)
        self.ctr_ops.setdefault(op.ctr, []).append(op)
        return op

    def op(self, eng, fn, reads=(), writes=()):
        return self._record(Op(eng, fn, eng, 1), reads, writes)

    def dma(self, eng, slot, fn, reads=(), writes=()):
        self.dma_slots.add(slot)
        o = Op(eng, fn, "dma:" + slot, 16)
        o.needed = True
        return self._record(o, reads, writes)

    def emit(self, final_waits=()):
        nc = self.nc
        ctrs = list(self.ctr_ops.keys())
        sems = {}
        for c in ctrs:
            sems[c] = (self.semstack or self.stack).enter_context(
                nc.semaphore("s_" + self.tag + c.replace(":", "_")))
        for c, ops in self.ctr_ops.items():
            v = 0
            for o in ops:
                if o.needed:
                    v += o.step
                    o.val = v
                else:
                    o.val = v
        engmap = {"pe": "tensor", "act": "scalar", "dve": "vector", "pool": "gpsimd", "sp": "sync"}
        fin = {}
        for o in final_waits:
            fin[o.ctr] = max(fin.get(o.ctr, 0), o.val)
        self.nwaits = 0
        with nc.Block() as block:
            for e in ENGS:
                ops = self.ops[e]
                last = (e == "sp")
                if not ops and not last:
                    continue

                def body(eng, ops=ops, e=e, last=last):
                    known = {}
                    for o in ops:
                        need = {}
                        for d in o.deps:
                            c, v = d if isinstance(d, tuple) else (d.ctr, d.val)
                            if v > need.get(c, 0):
                                need[c] = v
                        for c, v in need.items():
                            if known.get(c, 0) < v:
                                eng.wait_ge(sems[c], v)
                                known[c] = v
                                self.nwaits += 1
                        ins = o.fn(eng)
                        if o.needed:
                            ins.then_inc(sems[o.ctr], o.step)
                    if last:
                        for c, v in fin.items():
                            eng.wait_ge(sems[c], v)

                getattr(block, engmap[e])(body)

    def close(self):
        self.stack.close()


D = 1024
FF = 1024
KC = 8
SBK = 512
ALPHA = (2.0 * 2) ** 0.25
LN_EPS = 1e-5


def _ln_feature_major(b, P, zt, src_keys, ones_f, ps_a, ps_b, tmp, gb, out_fn, tag):
    sq, mean, var, rstd = tmp["a"], tmp["b"], tmp["c"], tmp["d"]
    for fc in range(KC):
        b.op("pe", lambda e, fc=fc: e.matmul(ps_a[:, :], ones_f[:, :], zt[:, fc, :],
                                            start=(fc == 0), stop=(fc == KC - 1)),
             reads=[("z", fc)], writes=[ps_a.name] if fc in (0, KC - 1) else [])
    for fc in range(KC):
        b.op("act", lambda e, fc=fc: e.activation(out=sq[:, :], in_=zt[:, fc, :], func=AF.Square),
             reads=[("z", fc)], writes=[sq.name])
        b.op("pe", lambda e, fc=fc: e.matmul(ps_b[:, :], ones_f[:, :], sq[:, :],
                                            start=(fc == 0), stop=(fc == KC - 1)),
             reads=[sq.name], writes=[ps_b.name] if fc in (0, KC - 1) else [])
    b.op("dve", lambda e: e.tensor_scalar(out=mean[:, :], in0=ps_a[:, :], scalar1=1.0 / D, scalar2=None,
                                          op0=ALU.mult), reads=[ps_a.name], writes=[mean.name])
    b.op("dve", lambda e: e.tensor_tensor(out=var[:, :], in0=mean[:, :], in1=mean[:, :], op=ALU.mult),
         reads=[mean.name], writes=[var.name])
    b.op("dve", lambda e: e.scalar_tensor_tensor(out=var[:, :], in0=ps_b[:, :], scalar=1.0 / D, in1=var[:, :],
                                                 op0=ALU.mult, op1=ALU.subtract),
         reads=[ps_b.name, var.name], writes=[var.name])
    b.op("dve", lambda e: e.tensor_scalar(out=var[:, :], in0=var[:, :], scalar1=LN_EPS, scalar2=None,
                                          op0=ALU.add), reads=[var.name], writes=[var.name])
    b.op("act", lambda e: e.activation(out=rstd[:, :], in_=var[:, :], func=AF.Sqrt),
         reads=[var.name], writes=[rstd.name])
    b.op("dve", lambda e: e.reciprocal(out=rstd[:, :], in_=rstd[:, :]), reads=[rstd.name], writes=[rstd.name])
    for fc in range(KC):
        b.op("dve", lambda e, fc=fc: e.tensor_tensor(out=zt[:, fc, :], in0=zt[:, fc, :], in1=mean[:, :],
                                                     op=ALU.subtract),
             reads=[("z", fc), mean.name], writes=[("z", fc)])
        b.op("pool", lambda e, fc=fc: e.tensor_tensor(out=zt[:, fc, :], in0=zt[:, fc, :], in1=rstd[:, :],
                                                      op=ALU.mult),
             reads=[("z", fc), rstd.name], writes=[("z", fc)])
        b.op("dve", lambda e, fc=fc: e.tensor_scalar(out=zt[:, fc, :], in0=zt[:, fc, :],
                                                     scalar1=gb[:, fc:fc + 1], scalar2=gb[:, 8 + fc:9 + fc],
                                                     op0=ALU.mult, op1=ALU.add),
             reads=[("z", fc), gb.name], writes=[("z", fc)])
        out_fn(fc)


def moe_phase(nc, semstack, tag, io, T=4096, NE=32, TB=1024):
    NTB = T // TB
    NSB = TB // SBK

    og = io["og"]; xres = io["xres"]; w_out = io["w_out"]; ln1 = io["ln1"]; ln2 = io["ln2"]
    rw = io["rw"]; rb = io["rb"]; wgu = io["wgu"]; bguT = io["bguT"]; wdn = io["wdn"]; bdn = io["bdn"]
    ident = io["ident"]; smask = io["smask"]; y_f32 = io["y_f32"]; y_b16 = io.get("y_b16")

    b = Builder(nc, semstack, tag)
    NU = 12
    ring = [b.sbuf(f"ring{i}", [128, 1024], BF16) for i in range(NU)]
    wo = b.sbuf("wo", [128, KC, D], BF16)
    acc = b.sbuf("acc", [128, KC, TB], F32)
    x1b = b.sbuf("x1b", [128, KC, TB], BF16)
    actp = [[b.sbuf(f"act{p}_{i}", [128, KC, SBK], BF16) for i in range(2)] for p in range(2)]
    actb = actp[0]
    z = b.sbuf("z", [128, KC, SBK], F32)
    tg = [b.sbuf(f"tg{i}", [128, SBK], F32) for i in range(2)]
    ts = [b.sbuf(f"ts{i}", [128, SBK], F32) for i in range(2)]
    tu = [b.sbuf(f"tu{i}", [128, SBK], F32) for i in range(2)]
    cbt = [[b.sbuf(f"cb{p}_{i}", [128, SBK], F32) for i in range(2)] for p in range(2)]
    lnt = {"a": tg[0], "b": ts[0], "c": tu[0], "d": tg[1]}
    ln1_sb = b.sbuf("ln1_sb", [128, 16], F32); ln2_sb = b.sbuf("ln2_sb", [128, 16], F32)
    rw_sb = b.sbuf("rw_sb", [128, KC, NE], F32); rb_sb = b.sbuf("rb_sb", [128, NE], F32)
    bgu_sb = b.sbuf("bgu_sb", [128, NE * 16], F32)
    bdn_sb = b.sbuf("bdn_sb", [NE, D], F32)
    cand = actp[1][0]
    sm_sb = b.sbuf("sm_sb", [128, 2], F32)
    zb = [b.sbuf(f"zb{i}", [128, SBK], BF16) for i in range(2)]
    id_sb = b.sbuf("id_sb", [128, 128], F32)
    ones_f = b.sbuf("ones_f", [128, 128], F32)
    combT = b.sbuf("combT", [NE, TB], F32)
    lg = b.sbuf("lg", [128, NE], F32); ex = b.sbuf("ex", [128, NE], F32); msk = b.sbuf("msk", [128, NE], F32)
    mx8 = b.sbuf("mx8", [128, 8], F32); nmx = b.sbuf("nmx", [128, 1], F32); ssum = b.sbuf("ssum", [128, 1], F32)

    ps_g = [b.psum(f"ps_g{i}", [128, SBK]) for i in range(2)]
    ps_u = [b.psum(f"ps_u{i}", [128, SBK]) for i in range(2)]
    ps_d = [b.psum(f"ps_d{i}", [128, SBK]) for i in range(2)]
    ps_c = b.psum("ps_c", [128, SBK])
    ps_m = b.psum("ps_m", [128, SBK])

    def ld(eng, slot, dst, src, key):
        b.dma(eng, slot, lambda e: e.dma_start(out=dst, in_=src), writes=[key])

    ld("sp", "c0", ln1_sb[:, :], ln1[:, :], ln1_sb.name)
    ld("sp", "c0", ln2_sb[:, :], ln2[:, :], ln2_sb.name)
    ld("sp", "c0", rw_sb[:, :, :], rw.rearrange("(kc p) n -> p kc n", p=128), rw_sb.name)
    ld("sp", "c0", rb_sb[:, :], rb[0:1, :].broadcast_to([128, NE]), rb_sb.name)
    ld("sp", "c0", bgu_sb[:, :], bguT[:, :], bgu_sb.name)
    ld("sp", "c0", bdn_sb[:, :], bdn[:, :], bdn_sb.name)
    ld("sp", "c0", sm_sb[:, :], smask[:, :], sm_sb.name)
    ld("sp", "c0", id_sb[:, :], ident[:, :], id_sb.name)
    b.op("dve", lambda e: e.memset(ones_f[:, :], 1.0), writes=[ones_f.name])
    bgu3 = bgu_sb[:, :].rearrange("p (e c) -> p e c", c=16)
    b.op("dve", lambda e: e.tensor_scalar(out=bgu3[:, :, 8:16], in0=bgu3[:, :, 8:16], scalar1=1.0, scalar2=None,
                                          op0=ALU.add), reads=[bgu_sb.name], writes=[bgu_sb.name])

    units = []
    for tb_ in range(NTB):
        for e_ in range(NE):
            for fc_ in range(KC):
                units.append(wgu[e_, 2 * fc_]); units.append(wgu[e_, 2 * fc_ + 1])
                if e_ >= 1:
                    units.append(wdn[e_ - 1, fc_])
        for fo_ in range(KC):
            units.append(wdn[NE - 1, fo_])
    st = {"loaded": 0, "n": 0}

    def _ensure(upto):
        while st["loaded"] < min(upto, len(units)):
            i = st["loaded"]
            u = ring[i % NU]
            src = units[i]
            b.dma("pool", "ring%d" % (i % NU), lambda e, u=u, src=src: e.dma_start(out=u[:, :], in_=src),
                  writes=[u.name])
            st["loaded"] += 1

    def next_unit():
        _ensure(st["n"] + 1)
        u = ring[st["n"] % NU]
        st["n"] += 1
        return u[:, :].rearrange("p (k c) -> p k c", k=KC), u.name

    def unit_done():
        _ensure(st["n"] + NU)

    b.dma("pool", "wo", lambda e: e.dma_start(out=wo[:, :, :], in_=w_out.rearrange("(kc p) f -> p kc f", p=128)),
          writes=[wo.name])

    out_ops = []
    for tb in range(NTB):
        t0 = tb * TB
        _ensure(st["n"] + NU)
        for sb in range(NSB):
            c0 = t0 + sb * SBK
            ob = actb[sb % 2]
            b.dma("pool", "ob" + str(sb % 2), lambda e, ob=ob, c0=c0: e.dma_start(
                out=ob[:, :, :], in_=og[:, :, :, c0:c0 + SBK].rearrange("i r p t -> p (i r) t")),
                writes=[(ob.name, k_) for k_ in range(KC)])
            b.dma("pool", "cand", lambda e, c0=c0: e.dma_start(
                out=cand[:, :, :], in_=og[:, :, :, T + c0:T + c0 + SBK].rearrange("i r p t -> p (i r) t")),
                writes=[(cand.name, k_) for k_ in range(KC)])
            b.op("dve", lambda e, ob=ob: e.tensor_scalar(out=ob[:, :, :], in0=ob[:, :, :], scalar1=sm_sb[:, 0:1],
                                                         scalar2=None, op0=ALU.mult),
                 reads=[(ob.name, k_) for k_ in range(KC)] + [sm_sb.name], writes=[(ob.name, k_) for k_ in range(KC)])
            b.op("dve", lambda e, ob=ob: e.scalar_tensor_tensor(out=ob[:, :, :], in0=cand[:, :, :], scalar=sm_sb[:, 1:2],
                                                                in1=ob[:, :, :], op0=ALU.mult, op1=ALU.add),
                 reads=[(ob.name, k_) for k_ in range(KC)] + [(cand.name, k_) for k_ in range(KC)] + [sm_sb.name],
                 writes=[(ob.name, k_) for k_ in range(KC)])
            for fc in range(KC):
                b.dma("sp", "zin", lambda e, fc=fc, c0=c0: e.dma_start(
                    out=z[:, fc, :], in_=xres[fc * 128:(fc + 1) * 128, c0:c0 + SBK]), writes=[("z", fc)])
            for fc in range(KC):
                pp = ps_d[fc % 2]
                for kc in range(KC):
                    b.op("pe", lambda e, pp=pp, kc=kc, fc=fc, ob=ob, wo=wo: e.matmul(
                        pp[:, :], wo[:, kc, fc * 128:(fc + 1) * 128], ob[:, kc, :],
                        start=(kc == 0), stop=(kc == KC - 1)),
                        reads=[wo.name, (ob.name, kc)], writes=[pp.name] if kc in (0, KC - 1) else [])
                b.op("dve", lambda e, pp=pp, fc=fc: e.scalar_tensor_tensor(
                    out=z[:, fc, :], in0=z[:, fc, :], scalar=ALPHA, in1=pp[:, :], op0=ALU.mult, op1=ALU.add),
                    reads=[("z", fc), pp.name], writes=[("z", fc)])

            def after_ln1(fc, sb=sb):
                cs = slice(sb * SBK, (sb + 1) * SBK)
                b.op("act", lambda e: e.activation(out=x1b[:, fc, cs], in_=z[:, fc, :], func=AF.Copy),
                     reads=[("z", fc)], writes=[("x1b", fc, sb)])
                b.op("pool", lambda e: e.tensor_scalar(out=acc[:, fc, cs], in0=z[:, fc, :], scalar1=ALPHA,
                                                       scalar2=None, op0=ALU.mult),
                     reads=[("z", fc)], writes=[("acc", fc, sb)])

            _ln_feature_major(b, None, z, None, ones_f, ps_c, ps_m, lnt, ln1_sb, after_ln1, "ln1")

            for tc in range(SBK // 128):
                for kc in range(KC):
                    b.op("pe", lambda e, tc=tc, kc=kc: e.matmul(
                        ps_c[:, 0:NE], z[:, kc, tc * 128:(tc + 1) * 128], rw_sb[:, kc, :],
                        start=(kc == 0), stop=(kc == KC - 1)),
                        reads=[("z", kc), rw_sb.name], writes=[ps_c.name] if kc in (0, KC - 1) else [])
                b.op("dve", lambda e: e.tensor_tensor(out=lg[:, :], in0=ps_c[:, 0:NE], in1=rb_sb[:, :], op=ALU.add),
                     reads=[ps_c.name, rb_sb.name], writes=[lg.name])
                b.op("dve", lambda e: e.max(out=mx8[:, :], in_=lg[:, :]), reads=[lg.name], writes=[mx8.name])
                b.op("dve", lambda e: e.tensor_scalar(out=msk[:, :], in0=lg[:, :], scalar1=mx8[:, 3:4], scalar2=None,
                                                      op0=ALU.is_ge), reads=[lg.name, mx8.name], writes=[msk.name])
                b.op("dve", lambda e: e.tensor_scalar(out=nmx[:, :], in0=mx8[:, 0:1], scalar1=-1.0, scalar2=None,
                                                      op0=ALU.mult), reads=[mx8.name], writes=[nmx.name])
                b.op("act", lambda e: e.activation(out=ex[:, :], in_=lg[:, :], func=AF.Exp, bias=nmx[:, 0:1], scale=1.0),
                     reads=[lg.name, nmx.name], writes=[ex.name])
                b.op("dve", lambda e: e.tensor_tensor(out=ex[:, :], in0=ex[:, :], in1=msk[:, :], op=ALU.mult),
                     reads=[ex.name, msk.name], writes=[ex.name])
                b.op("dve", lambda e: e.reduce_sum(out=ssum[:, :], in_=ex[:, :], axis=AX.X),
                     reads=[ex.name], writes=[ssum.name])
                b.op("dve", lambda e: e.reciprocal(out=ssum[:, :], in_=ssum[:, :]), reads=[ssum.name], writes=[ssum.name])
                b.op("dve", lambda e: e.tensor_scalar(out=ex[:, :], in0=ex[:, :], scalar1=ssum[:, 0:1], scalar2=None,
                                                      op0=ALU.mult), reads=[ex.name, ssum.name], writes=[ex.name])
                b.op("pe", lambda e: e.transpose(out=ps_m[0:NE, 0:128], in_=ex[:, :], identity=id_sb[:, :]),
                     reads=[ex.name, id_sb.name], writes=[ps_m.name])
                cofs = sb * SBK + tc * 128
                b.op("act", lambda e, cofs=cofs: e.activation(out=combT[:, cofs:cofs + 128], in_=ps_m[0:NE, 0:128],
                                                              func=AF.Copy),
                     reads=[ps_m.name], writes=[("combT", sb, tc)])
            cs = slice(sb * SBK, (sb + 1) * SBK)
            for fo in range(KC):
                pp = ps_d[fo % 2]
                b.op("pe", lambda e, pp=pp, fo=fo, cs=cs: e.matmul(pp[:, :], bdn_sb[:, fo * 128:(fo + 1) * 128],
                                                                  combT[:, cs], start=True, stop=True),
                     reads=[bdn_sb.name] + [("combT", sb, tc) for tc in range(4)], writes=[pp.name])
                b.op("dve", lambda e, pp=pp, fo=fo, cs=cs: e.tensor_tensor(out=acc[:, fo, cs], in0=acc[:, fo, cs],
                                                                          in1=pp[:, :], op=ALU.add),
                     reads=[("acc", fo, sb), pp.name], writes=[("acc", fo, sb)])

        tile_q = []
        tcount = [0]

        def stage2(item):
            g_t, s_t, u_t, cb, ab, fc = item
            b.op("dve", lambda e: e.tensor_scalar(out=u_t[:, :], in0=u_t[:, :], scalar1=-6.0, scalar2=8.0,
                                                  op0=ALU.max, op1=ALU.min), reads=[u_t.name], writes=[u_t.name])
            b.op("dve", lambda e: e.tensor_tensor(out=u_t[:, :], in0=u_t[:, :], in1=cb[:, :], op=ALU.mult),
                 reads=[u_t.name, cb.name], writes=[u_t.name])
            b.op("dve", lambda e: e.tensor_tensor(out=g_t[:, :], in0=g_t[:, :], in1=s_t[:, :], op=ALU.mult),
                 reads=[g_t.name, s_t.name], writes=[g_t.name])
            b.op("dve", lambda e: e.tensor_tensor(out=ab[:, fc, :], in0=g_t[:, :], in1=u_t[:, :], op=ALU.mult),
                 reads=[g_t.name, u_t.name], writes=[(ab.name, fc)])

        def down(ex_d, fo):
            Dn, Dkey = next_unit()
            for sb in range(NSB):
                cs = slice(sb * SBK, (sb + 1) * SBK)
                ab = actp[ex_d % 2][sb]
                pd = ps_d[sb % 2]
                for fc in range(KC):
                    b.op("pe", lambda e, pd=pd, fc=fc, ab=ab, Dn=Dn: e.matmul(
                        pd[:, :], Dn[:, fc, :], ab[:, fc, :], start=(fc == 0), stop=(fc == KC - 1)),
                        reads=[Dkey, (ab.name, fc)], writes=[pd.name] if fc in (0, KC - 1) else [])
                b.op("dve", lambda e, pd=pd, fo=fo, cs=cs: e.tensor_tensor(out=acc[:, fo, cs], in0=acc[:, fo, cs],
                                                                          in1=pd[:, :], op=ALU.add),
                     reads=[("acc", fo, sb), pd.name], writes=[("acc", fo, sb)])
            unit_done()

        for ex_i in range(NE):
            par = ex_i % 2
            for sb in range(NSB):
                cs = slice(sb * SBK, (sb + 1) * SBK)
                cb = cbt[par][sb]
                b.op("pe", lambda e, cs=cs, ex_i=ex_i: e.matmul(ps_c[:, :], id_sb[0:NE, ex_i:ex_i + 1].broadcast_to([NE, 128]),
                                                               combT[:, cs], start=True, stop=True),
                     reads=[id_sb.name] + [("combT", sb, tc) for tc in range(4)], writes=[ps_c.name])
                b.op("act", lambda e, cb=cb: e.activation(out=cb[:, :], in_=ps_c[:, :], func=AF.Copy),
                     reads=[ps_c.name], writes=[cb.name])
            for fc in range(KC):
                G, Gkey = next_unit()
                U, Ukey = next_unit()
                for W, Wkey, pz in ((G, Gkey, ps_g), (U, Ukey, ps_u)):
                    for kc in range(KC):
                        for sb in range(NSB):
                            cs = slice(sb * SBK, (sb + 1) * SBK)
                            b.op("pe", lambda e, W=W, pz=pz, kc=kc, sb=sb, cs=cs: e.matmul(
                                pz[sb][:, :], W[:, kc, :], x1b[:, kc, cs], start=(kc == 0), stop=(kc == KC - 1)),
                                reads=[Wkey, ("x1b", kc, sb)], writes=[pz[sb].name] if kc in (0, KC - 1) else [])
                unit_done()
                bg = bgu_sb[:, ex_i * 16 + fc: ex_i * 16 + fc + 1]
                bu = bgu_sb[:, ex_i * 16 + 8 + fc: ex_i * 16 + 8 + fc + 1]
                for sb in range(NSB):
                    ti = tcount[0] % 2
                    tcount[0] += 1
                    g_t = tg[ti]; s_t = ts[ti]; u_t = tu[ti]
                    pg = ps_g[sb]; pu = ps_u[sb]
                    b.op("dve", lambda e, pg=pg, g_t=g_t, bg=bg: e.tensor_scalar(
                        out=g_t[:, :], in0=pg[:, :], scalar1=bg, scalar2=7.0, op0=ALU.add, op1=ALU.min),
                        reads=[pg.name, bgu_sb.name], writes=[g_t.name])
                    b.op("act", lambda e, g_t=g_t, s_t=s_t: e.activation(out=s_t[:, :], in_=g_t[:, :],
                                                                         func=AF.Sigmoid, scale=1.702),
                         reads=[g_t.name], writes=[s_t.name])
                    b.op("act", lambda e, pu=pu, u_t=u_t, bu=bu: e.activation(out=u_t[:, :], in_=pu[:, :],
                                                                             func=AF.Identity, bias=bu, scale=1.0),
                         reads=[pu.name, bgu_sb.name], writes=[u_t.name])
                    if tile_q:
                        stage2(tile_q.pop(0))
                    tile_q.append((g_t, s_t, u_t, cbt[par][sb], actp[par][sb], fc))
                if ex_i >= 1:
                    down(ex_i - 1, fc)
        while tile_q:
            stage2(tile_q.pop(0))
        for fo in range(KC):
            down(NE - 1, fo)

        for sb in range(NSB):
            cs = slice(sb * SBK, (sb + 1) * SBK)
            c0 = t0 + sb * SBK
            for fc in range(KC):
                b.op("act", lambda e, fc=fc, cs=cs: e.activation(out=z[:, fc, :], in_=acc[:, fc, cs], func=AF.Copy),
                     reads=[("acc", fc, sb)], writes=[("z", fc)])

            def after_ln2(fc, c0=c0):
                o = b.dma("sp", "yout", lambda e: e.dma_start(out=y_f32[fc * 128:(fc + 1) * 128, c0:c0 + SBK],
                                                              in_=z[:, fc, :]), reads=[("z", fc)])
                out_ops.append(o)
                if y_b16 is not None:
                    zb_ = zb[fc % 2]
                    b.op("act", lambda e: e.activation(out=zb_[:, :], in_=z[:, fc, :], func=AF.Copy),
                         reads=[("z", fc)], writes=[zb_.name])
                    o2 = b.dma("sp", "youtb%d" % (fc % 2), lambda e: e.dma_start(
                        out=y_b16[fc // 2, (fc % 2) * 128:(fc % 2 + 1) * 128, c0:c0 + SBK], in_=zb_[:, :]),
                        reads=[zb_.name])
                    out_ops.append(o2)

            _ln_feature_major(b, None, z, None, ones_f, ps_c, ps_m, lnt, ln2_sb, after_ln2, "ln2")

    b.emit(final_waits=out_ops)
    b.close()


def attn_phase(nc, semstack, tag, io, S=8192):
    NQB = None
    NTB = S // SBK
    NKB = S // 128
    NRP = S // 128
    if NQB is None:
        NQB = NTB

    w_sel = io["w_sel"]; t5c = io["t5c"]; t5b = io["t5b"]; lamv = io["lamv"]; cst = io["cst"]
    gsub = io["gsub"]; nab = io["nab"]; o_scr = io["o_scr"]; x_src = io["x_src"]

    b = Builder(nc, semstack, tag)
    BQ = [b.sbuf(f"BQ{i}", [128, S], BF16) for i in range(2)]
    BK = [b.sbuf(f"BK{i}", [128, S], BF16) for i in range(2)]
    BV = b.sbuf("BV", [128, NKB, 256], BF16)
    wsb = b.sbuf("wsb", [128, KC, 768], BF16)
    xb = [b.sbuf(f"xb{i}", [128, KC, SBK], BF16) for i in range(2)]
    t5b_sb = b.sbuf("t5b_sb", [128, 12, SBK], F32)
    nab_sb = [b.sbuf(f"nab{i}", [128, 640], F32) for i in range(2)]
    t5c_sb = b.sbuf("t5c_sb", [128, 4], F32)
    lam_sb = b.sbuf("lam_sb", [128, 256], F32); cst_sb = b.sbuf("cst_sb", [128, 4], F32)
    gcol = b.sbuf("gcol", [128, 1], F32); nlam = b.sbuf("nlam", [128, 1], F32)
    e12 = b.sbuf("e12", [128, 2], F32); lprod = b.sbuf("lprod", [128, 128], F32)
    ones_b = b.sbuf("ones_b", [128, 128], BF16); ones_f = b.sbuf("ones_f", [128, 128], F32)
    tA = b.sbuf("tA", [128, SBK], F32); tB = b.sbuf("tB", [128, SBK], F32)
    tAo = [b.sbuf(f"tAo{i}", [128, SBK], BF16) for i in range(2)]
    nsb = [b.sbuf(f"nsb{i}", [128, 640], F32) for i in range(2)]
    nP = [b.sbuf(f"nP{i}", [128, 640], BF16) for i in range(2)]
    nr = b.sbuf("nr", [64, 128], F32)
    nout = [b.sbuf(f"nout{i}", [64, SBK], BF16) for i in range(2)]

    class _Bank:
        def __init__(self, t, j):
            self.t = t; self.j = j; self.name = t.name + "_b%d" % j

        def __getitem__(self, idx):
            return self.t[:, self.j, :][idx]

    PP = [b.psum(f"PP{i}", [128, 2, SBK]) for i in range(4)]
    psb = [_Bank(PP[i // 2], i % 2) for i in range(8)]
    Pt2 = [b.sbuf(f"Pt2_{i}", [128, 2, SBK], BF16) for i in range(3)]
    dstep = [0]
    tsp2 = b.sbuf("tsp2", [128, 2, SBK], F32)
    dacc = b.sbuf("dacc", [128, 2, SBK], F32)

    def ld(eng, slot, dst, src, key):
        b.dma(eng, slot, lambda e: e.dma_start(out=dst, in_=src), writes=[key])

    ld("sp", "c0", t5c_sb[:, :], t5c[:, :], t5c_sb.name)
    ld("sp", "c0", lam_sb[:, :], lamv[0:1, :].broadcast_to([128, 256]), lam_sb.name)
    ld("sp", "c0", cst_sb[:, :], cst[:, :], cst_sb.name)
    ld("sp", "c0", gcol[:, :], gsub[:, :], gcol.name)
    for h in range(2):
        for v in range(6):
            ld("sp", "c0", t5b_sb[:, h * 6 + v, :], t5b[h, v, :, :], t5b_sb.name)
    b.op("dve", lambda e: e.memset(ones_f[:, :], 1.0), writes=[ones_f.name])
    b.op("dve", lambda e: e.memset(ones_b[:, :], 1.0), writes=[ones_b.name])
    b.op("dve", lambda e: e.tensor_tensor(out=lprod[:, 0:64], in0=lam_sb[:, 0:64], in1=lam_sb[:, 64:128], op=ALU.mult),
         reads=[lam_sb.name], writes=[lprod.name])
    b.op("dve", lambda e: e.tensor_tensor(out=lprod[:, 64:128], in0=lam_sb[:, 128:192], in1=lam_sb[:, 192:256], op=ALU.mult),
         reads=[lam_sb.name, lprod.name], writes=[lprod.name])
    b.op("dve", lambda e: e.reduce_sum(out=e12[:, 0:1], in_=lprod[:, 0:64], axis=AX.X), reads=[lprod.name], writes=[e12.name])
    b.op("dve", lambda e: e.reduce_sum(out=e12[:, 1:2], in_=lprod[:, 64:128], axis=AX.X), reads=[lprod.name, e12.name], writes=[e12.name])
    b.op("act", lambda e: e.activation(out=e12[:, :], in_=e12[:, :], func=AF.Exp), reads=[e12.name], writes=[e12.name])
    b.op("dve", lambda e: e.tensor_tensor(out=nlam[:, :], in0=e12[:, 1:2], in1=e12[:, 0:1], op=ALU.subtract),
         reads=[e12.name], writes=[nlam.name])
    b.op("dve", lambda e: e.tensor_tensor(out=nlam[:, :], in0=nlam[:, :], in1=cst_sb[:, 0:1], op=ALU.subtract),
         reads=[nlam.name, cst_sb.name], writes=[nlam.name])
    b.op("dve", lambda e: e.tensor_tensor(out=gcol[:, :], in0=gcol[:, :], in1=cst_sb[:, 1:2], op=ALU.mult),
         reads=[gcol.name, cst_sb.name], writes=[gcol.name])

    out_ops = []

    def in_proj(col0):
        b.dma("pool", "wsb", lambda e: e.dma_start(
            out=wsb[:, :, :], in_=w_sel[:, col0:col0 + 768].rearrange("(kc p) f -> p kc f", p=128)),
            writes=[wsb.name])
        for tb in range(NTB):
            x_ = xb[tb % 2]
            if io["x_3d"]:
                b.dma("pool", "xb%d" % (tb % 2), lambda e, x_=x_, tb=tb: e.dma_start(
                    out=x_[:, :, :], in_=x_src(tb)), writes=[x_.name])
            else:
                for j in range(2):
                    b.dma("pool", "xb%d" % (tb % 2), lambda e, x_=x_, tb=tb, j=j: e.dma_start(
                        out=x_[:, :, :].rearrange("p (i j) t -> p i j t", i=4)[:, :, j, :],
                        in_=x_src(tb, j)), writes=[x_.name])
            for oc in range(4):
                pp = psb[oc % 2]
                for kc in range(KC):
                    b.op("pe", lambda e, pp=pp, kc=kc, oc=oc, x_=x_: e.matmul(
                        pp[:, :], wsb[:, kc, oc * 128:(oc + 1) * 128], x_[:, kc, :],
                        start=(kc == 0), stop=(kc == KC - 1)),
                        reads=[wsb.name, x_.name], writes=[pp.name] if kc in (0, KC - 1) else [])
                dst = (BQ if oc < 2 else BK)[oc % 2]
                sc = 0.125 if oc < 2 else 1.0
                eng = "act" if oc % 2 == 0 else "dve"
                if eng == "act":
                    b.op("act", lambda e, pp=pp, dst=dst, tb=tb, sc=sc: e.activation(
                        out=dst[:, tb * SBK:(tb + 1) * SBK], in_=pp[:, :], func=AF.Copy, scale=sc),
                        reads=[pp.name], writes=[(dst.name, tb)])
                else:
                    b.op("dve", lambda e, pp=pp, dst=dst, tb=tb, sc=sc: e.tensor_scalar(
                        out=dst[:, tb * SBK:(tb + 1) * SBK], in0=pp[:, :], scalar1=sc, scalar2=None, op0=ALU.mult),
                        reads=[pp.name], writes=[(dst.name, tb)])
            for tc in range(4):
                pp = psb[2 + tc % 2]
                for kc in range(KC):
                    b.op("pe", lambda e, pp=pp, kc=kc, tc=tc, x_=x_: e.matmul(
                        pp[:, 0:256], x_[:, kc, tc * 128:(tc + 1) * 128], wsb[:, kc, 512:768],
                        start=(kc == 0), stop=(kc == KC - 1)),
                        reads=[wsb.name, x_.name], writes=[pp.name] if kc in (0, KC - 1) else [])
                ch = tb * 4 + tc
                if tc % 2 == 0:
                    b.op("act", lambda e, pp=pp, ch=ch: e.activation(out=BV[:, ch, :], in_=pp[:, 0:256], func=AF.Copy),
                         reads=[pp.name], writes=[("BV", ch)])
                else:
                    b.op("dve", lambda e, pp=pp, ch=ch: e.tensor_copy(out=BV[:, ch, :], in_=pp[:, 0:256]),
                         reads=[pp.name], writes=[("BV", ch)])

    in_proj(0)
    for h in range(2):
        for qb in range(NQB):
            qs = slice(qb * SBK, (qb + 1) * SBK)
            po = [psb[4], psb[5]]; pd = [psb[6], psb[7]]

            def qk(kb, h=h, qb=qb, qs=qs):
                for m in range(2):
                    pp = psb[(kb % 2) * 2 + m]
                    b.op("pe", lambda e, pp=pp, m=m: e.matmul(
                        pp[:, :], BK[h][m * 64:(m + 1) * 64, kb * 128:(kb + 1) * 128], BQ[h][m * 64:(m + 1) * 64, qs],
                        start=True, stop=True),
                        reads=[(BK[h].name, kb // 4), (BQ[h].name, qb)], writes=[pp.name])

            def expav(kb, h=h, qb=qb):
                rel = kb - 4 * qb
                PPk = PP[kb % 2]
                pk = [psb[(kb % 2) * 2].name, psb[(kb % 2) * 2 + 1].name]
                P = Pt2[dstep[0] % 3]
                dstep[0] += 1
                if -1 <= rel <= 4:
                    bt = t5b_sb[:, h * 6 + rel + 1, :]
                    for m in range(2):
                        b.op("dve", lambda e, m=m, bt=bt, PPk=PPk: e.tensor_tensor(
                            out=tsp2[:, m, :], in0=PPk[:, m, :], in1=bt, op=ALU.add),
                            reads=[pk[m], t5b_sb.name], writes=[(tsp2.name, m)])
                    b.op("act", lambda e, P=P: e.activation(out=P[:, :, :], in_=tsp2[:, :, :], func=AF.Exp),
                         reads=[(tsp2.name, 0), (tsp2.name, 1)], writes=[P.name])
                else:
                    col = h * 2 + (0 if rel < 0 else 1)
                    b.op("act", lambda e, PPk=PPk, P=P, col=col: e.activation(
                        out=P[:, :, :], in_=PPk[:, :, :], func=AF.Exp, bias=t5c_sb[:, col:col + 1], scale=1.0),
                        reads=pk + [t5c_sb.name], writes=[P.name])
                for m in range(2):
                    b.op("pe", lambda e, P=P, m=m: e.matmul(
                        po[m][:, :], BV[:, kb, h * 128:(h + 1) * 128], P[:, m, :], start=(kb == 0), stop=(kb == NKB - 1)),
                        reads=[("BV", kb), P.name], writes=[po[m].name] if kb in (0, NKB - 1) else [])
                if kb == 0:
                    b.op("dve", lambda e, P=P: e.tensor_copy(out=dacc[:, :, :], in_=P[:, :, :]),
                         reads=[P.name], writes=[dacc.name])
                else:
                    b.op("dve", lambda e, P=P: e.tensor_tensor(out=dacc[:, :, :], in0=dacc[:, :, :], in1=P[:, :, :], op=ALU.add),
                         reads=[P.name, dacc.name], writes=[dacc.name])

            qk(0)
            for kb in range(NKB):
                if kb + 1 < NKB:
                    qk(kb + 1)
                expav(kb)
            for m in range(2):
                b.op("pe", lambda e, m=m: e.matmul(pd[m][:, :], ones_f[:, :], dacc[:, m, :], start=True, stop=True),
                     reads=[ones_f.name, dacc.name], writes=[pd[m].name])
            b.op("dve", lambda e: e.reciprocal(out=tA[:, :], in_=pd[0][:, :]), reads=[pd[0].name], writes=[tA.name])
            b.op("dve", lambda e: e.tensor_tensor(out=tA[:, :], in0=po[0][:, :], in1=tA[:, :], op=ALU.mult),
                 reads=[po[0].name, tA.name], writes=[tA.name])
            b.op("dve", lambda e: e.reciprocal(out=tB[:, :], in_=pd[1][:, :]), reads=[pd[1].name], writes=[tB.name])
            b.op("dve", lambda e: e.tensor_tensor(out=tB[:, :], in0=po[1][:, :], in1=tB[:, :], op=ALU.mult),
                 reads=[po[1].name, tB.name], writes=[tB.name])
            b.op("dve", lambda e: e.scalar_tensor_tensor(out=tA[:, :], in0=tB[:, :], scalar=nlam[:, 0:1], in1=tA[:, :],
                                                         op0=ALU.mult, op1=ALU.add),
                 reads=[tA.name, tB.name, nlam.name], writes=[tA.name])
            b.op("act", lambda e: e.activation(out=tB[:, :], in_=tA[:, :], func=AF.Square), reads=[tA.name], writes=[tB.name])
            pn = psb[0]
            b.op("pe", lambda e: e.matmul(pn[:, :], ones_f[:, :], tB[:, :], start=True, stop=True),
                 reads=[ones_f.name, tB.name], writes=[pn.name])
            b.op("dve", lambda e: e.tensor_scalar(out=tB[:, :], in0=pn[:, :], scalar1=1.0 / 128, scalar2=1e-5,
                                                  op0=ALU.mult, op1=ALU.add), reads=[pn.name], writes=[tB.name])
            b.op("act", lambda e: e.activation(out=tB[:, :], in_=tB[:, :], func=AF.Sqrt), reads=[tB.name], writes=[tB.name])
            b.op("dve", lambda e: e.reciprocal(out=tB[:, :], in_=tB[:, :]), reads=[tB.name], writes=[tB.name])
            b.op("dve", lambda e: e.tensor_tensor(out=tA[:, :], in0=tA[:, :], in1=tB[:, :], op=ALU.mult),
                 reads=[tA.name, tB.name], writes=[tA.name])
            to_ = tAo[qb % 2]
            b.op("dve", lambda e, to_=to_: e.tensor_scalar(out=to_[:, :], in0=tA[:, :], scalar1=gcol[:, 0:1], scalar2=None, op0=ALU.mult),
                 reads=[tA.name, gcol.name], writes=[to_.name])
            o = b.dma("sp", "oda%d" % (qb % 2), lambda e, h=h, qs=qs, to_=to_: e.dma_start(out=o_scr[h, :, qs], in_=to_[:, :]),
                      reads=[to_.name])
            out_ops.append(o)

    in_proj(768)
    NRPQ = NRP if NQB == NTB else NQB * 4
    na_units = []
    for n in range(4):
        for rp in range(NRPQ):
            na_units.append((n, rp))

    def na_qk(ui):
        n, rp = na_units[ui]
        p = n // 2
        pb = (n % 2) * 64
        cs = min(max(rp - 2, 0), NRP - 5)
        if rp == 0:
            v = 1
        elif rp == 1:
            v = 2
        elif rp == NRP - 2:
            v = 3
        elif rp == NRP - 1:
            v = 4
        else:
            v = 0
        i2 = ui % 2
        nb_ = nab_sb[i2]
        b.dma("sp", "nab%d" % i2, lambda e, nb_=nb_, n=n, v=v: e.dma_start(out=nb_[:, :], in_=nab[n, v, :, :]),
              writes=[nb_.name])
        psA = psb[i2 * 2]; psB = psb[i2 * 2 + 1]
        for c in range(5):
            dst = psA[:, c * 128:(c + 1) * 128] if c < 4 else psB[:, 0:128]
            b.op("pe", lambda e, dst=dst, c=c, p=p, pb=pb, cs=cs, rp=rp: e.matmul(
                dst, BK[p][pb:pb + 64, (cs + c) * 128:(cs + c + 1) * 128], BQ[p][pb:pb + 64, rp * 128:(rp + 1) * 128],
                start=True, stop=True),
                reads=[(BK[p].name, (cs + c) // 4), (BQ[p].name, rp // 4)],
                writes=[psA.name if c < 4 else psB.name])

    def na_rest(ui):
        n, rp = na_units[ui]
        cs = min(max(rp - 2, 0), NRP - 5)
        i2 = ui % 2
        nb_ = nab_sb[i2]
        psA = psb[i2 * 2]; psB = psb[i2 * 2 + 1]
        ppo = psb[4 + i2]; ppd = psb[6 + i2]
        sb_ = nsb[i2]; P = nP[i2]
        b.op("dve", lambda e: e.tensor_tensor(out=sb_[:, 0:512], in0=psA[:, :], in1=nb_[:, 0:512], op=ALU.add),
             reads=[psA.name, nb_.name], writes=[(sb_.name, 0)])
        b.op("dve", lambda e: e.tensor_tensor(out=sb_[:, 512:640], in0=psB[:, 0:128], in1=nb_[:, 512:640], op=ALU.add),
             reads=[psB.name, nb_.name], writes=[(sb_.name, 1)])
        b.op("act", lambda e: e.activation(out=P[:, :], in_=sb_[:, :], func=AF.Exp),
             reads=[(sb_.name, 0), (sb_.name, 1)], writes=[P.name])
        for c in range(5):
            b.op("pe", lambda e, c=c: e.matmul(
                ppo[0:64, 0:128], BV[:, cs + c, n * 64:(n + 1) * 64], P[:, c * 128:(c + 1) * 128],
                start=(c == 0), stop=(c == 4)),
                reads=[("BV", cs + c), P.name], writes=[ppo.name] if c in (0, 4) else [])
        for c in range(5):
            b.op("pe", lambda e, c=c: e.matmul(
                ppd[0:64, 0:128], ones_b[:, 0:64], P[:, c * 128:(c + 1) * 128], start=(c == 0), stop=(c == 4)),
                reads=[ones_b.name, P.name], writes=[ppd.name] if c in (0, 4) else [])
        no = nout[(rp // 4) % 2]
        b.op("dve", lambda e: e.reciprocal(out=nr[:, :], in_=ppd[0:64, 0:128]), reads=[ppd.name], writes=[nr.name])
        b.op("dve", lambda e: e.tensor_tensor(
            out=no[:, (rp % 4) * 128:(rp % 4 + 1) * 128], in0=ppo[0:64, 0:128], in1=nr[:, :], op=ALU.mult),
            reads=[ppo.name, nr.name], writes=[(no.name, rp % 4)])
        if rp % 4 == 3:
            r0 = (rp // 4) * SBK
            o = b.dma("sp", "ona%d" % ((rp // 4) % 2), lambda e: e.dma_start(
                out=o_scr[2 + n // 2, (n % 2) * 64:(n % 2 + 1) * 64, r0:r0 + SBK], in_=no[:, :]),
                reads=[(no.name, i) for i in range(4)])
            out_ops.append(o)

    na_qk(0)
    for ui in range(len(na_units)):
        if ui + 1 < len(na_units):
            na_qk(ui + 1)
        na_rest(ui)

    b.emit(final_waits=out_ops)
    b.close()


import math as _math

HEAD_DIM = 64
GRID_W = 64
NEG = -30000.0


def _t5_bucket_np(rel):
    rel = np.asarray(rel, np.int32)
    half = 16
    max_exact = 8
    ret = (rel > 0).astype(np.int32) * half
    n = np.abs(rel)
    n_f = np.maximum(n, 1).astype(np.float32)
    large = max_exact + (np.log(n_f / np.float32(max_exact)) / np.float32(_math.log(128 / max_exact))
                         * np.float32(half - max_exact)).astype(np.int32)
    large = np.minimum(large, half - 1)
    return ret + np.where(n < max_exact, n, large)


def _t5_tiles_idx():
    p = np.arange(128)[:, None]
    j = np.arange(SBK)[None, :]
    return np.stack([_t5_bucket_np((v - 1) * 128 + p - j) for v in range(6)])


def _na_tiles_idx(rows):
    nrp = rows // 2
    reps = {0: 2 if nrp > 4 else None, 1: 0, 2: 1, 3: nrp - 2, 4: nrp - 1}
    col_start = np.clip(np.arange(GRID_W) - 8, 0, GRID_W - 16)
    ro = np.zeros((5, 128, 640), np.int64); co = np.zeros((5, 128, 640), np.int64)
    va = np.zeros((5, 128, 640), bool)
    kic = np.arange(128)
    q = np.arange(128)
    for v, rp in reps.items():
        if rp is None:
            continue
        cs = min(max(rp - 2, 0), nrp - 5)
        for c in range(5):
            krow = 2 * (cs + c) + kic // 64
            kcol = kic % 64
            r = 2 * rp + q // 64
            w = q % 64
            rs = np.clip(r - 4, 0, rows - 8)
            valid = ((krow[:, None] >= rs[None, :]) & (krow[:, None] < rs[None, :] + 8) &
                     (kcol[:, None] >= col_start[w][None, :]) & (kcol[:, None] < col_start[w][None, :] + 16))
            roff = krow[:, None] - r[None, :] + 7
            coff = kcol[:, None] - w[None, :] + 15
            sl = slice(c * 128, (c + 1) * 128)
            va[v, :, sl] = valid
            ro[v, :, sl] = np.where(valid, roff, 0)
            co[v, :, sl] = np.where(valid, coff, 0)
    return ro, co, va


def prep_attn_inputs(x_b, w_in_l, hh, layer, lq1, lk1, lq2, lk2, subln_g, t5_table, na_rpb_l, S=None):
    f = np.float32
    S = x_b.shape[0] if S is None else S
    dah = [2 * hh, 2 * hh + 1]
    nah = [4 * hh + i for i in range(4)]
    o_q1, o_q2, o_k1, o_k2, o_va, o_qn, o_kn, o_vn = 0, 256, 512, 768, 1024, 1536, 2048, 2560
    cols = []
    for h in dah:
        cols += list(range(o_q1 + 64 * h, o_q1 + 64 * h + 64)) + list(range(o_q2 + 64 * h, o_q2 + 64 * h + 64))
    for h in dah:
        cols += list(range(o_k1 + 64 * h, o_k1 + 64 * h + 64)) + list(range(o_k2 + 64 * h, o_k2 + 64 * h + 64))
    for h in dah:
        cols += list(range(o_va + 128 * h, o_va + 128 * h + 128))
    for base in (o_qn, o_kn, o_vn):
        for n in nah:
            cols += list(range(base + 64 * n, base + 64 * n + 64))
    w_sel = np.ascontiguousarray(w_in_l[:, cols]).astype(f)
    t5c = np.empty((128, 4), f)
    for i, h in enumerate(dah):
        t5c[:, 2 * i] = t5_table[15, h]
        t5c[:, 2 * i + 1] = t5_table[31, h]
    tidx = _t5_tiles_idx()
    t5b = np.stack([t5_table[:, h][tidx] for h in dah]).astype(f)
    lamv = np.concatenate([lq1, lk1, lq2, lk2]).astype(f)[None, :]
    lam_init = 0.8 - 0.6 * _math.exp(-0.3 * layer)
    cst = np.zeros((128, 4), f); cst[:, 0] = lam_init; cst[:, 1] = 1.0 - lam_init
    ro, co, va = _na_tiles_idx(S // GRID_W)
    nab = np.stack([np.where(va, na_rpb_l[n][ro, co], f(NEG)) for n in nah]).astype(f)
    return dict(w_sel=w_sel, t5c=t5c, t5b=t5b, lamv=lamv, cst=cst,
                gsub=np.ascontiguousarray(subln_g.astype(f)[:, None]), nab=nab)


def _pack_gb(g, bb):
    return np.ascontiguousarray(np.concatenate([g.reshape(8, 128).T, bb.reshape(8, 128).T], axis=1)).astype(np.float32)


PAIRS = [[0, 1], [2, 3], [4, 5], [6, 7]]


def xchg_phase(nc, semstack, tag, pairs):
    sem = semstack.enter_context(nc.semaphore("cc_" + tag))
    with nc.Block() as block:
        @block.gpsimd
        def _(g):
            for src, dst in pairs:
                g.collective_compute("AllGather", ALU.bypass, replica_groups=PAIRS,
                                     ins=[src], outs=[dst]).then_inc(sem)
            g.wait_ge(sem, len(pairs))


def build_fused(S=8192, NE=32, depth=2, TB=1024):
    nc = bass.Bass("TRN2", target_bir_lowering=False)
    T = S // 2

    def din(name, shape):
        return nc.dram_tensor(name, list(shape), F32, kind="ExternalInput").ap()

    xT_full = din("xT_full", [D, S]); xT_own = din("xT_own", [D, T]); smask = din("smask", [128, 2])
    w_sel = din("w_sel", [depth, D, 1536]); t5c = din("t5c", [128, 4]); t5b = din("t5b", [2, 6, 128, SBK])
    lamv = din("lamv", [depth, 256]); cst = din("cst", [depth, 128, 4]); gsub = din("gsub", [depth, 128, 1])
    nab = din("nab", [depth, 4, 5, 128, 640])
    w_out = din("w_out", [depth, D, D]); ln1 = din("ln1", [depth, 128, 16]); ln2 = din("ln2", [depth, 128, 16])
    rw = din("rw", [depth, D, NE]); rb = din("rb", [depth, NE])
    wgu = din("wgu", [depth, NE, 16, 128, 1024]); bguT = din("bguT", [depth, 128, NE * 16])
    wdn = din("wdn", [depth, NE, 8, 128, 1024]); bdn = din("bdn", [depth, NE, D])
    ident = din("ident", [128, 128])
    yT = nc.dram_tensor("yT", [D, T], F32, kind="ExternalOutput").ap()
    o_scr = nc.dram_tensor("o_scr", [4, 128, S], BF16).ap()
    og = nc.dram_tensor("og", [4, 2, 128, S], BF16).ap()
    x1_scr = nc.dram_tensor("x1_scr", [D, T], F32).ap()
    xb_scr = nc.dram_tensor("xb_scr", [4, 256, T], BF16).ap()
    xg = nc.dram_tensor("xg", [4, 2, 2, 128, T], BF16).ap()

    semstack = contextlib.ExitStack()
    nblk = T // SBK
    for l in range(depth):
        if l == 0:
            x_src = lambda tb: xT_full[:, tb * SBK:(tb + 1) * SBK].rearrange("(kc p) t -> p kc t", p=128)
        else:
            x_src = lambda tb, j: xg[:, tb // nblk, j, :, (tb % nblk) * SBK:(tb % nblk + 1) * SBK].rearrange(
                "i p t -> p i t")
        attn_phase(nc, semstack, "a%d_" % l, dict(
            w_sel=w_sel[l], t5c=t5c, t5b=t5b, lamv=lamv[l:l + 1, :], cst=cst[l], gsub=gsub[l], nab=nab[l],
            o_scr=o_scr, x_src=x_src, x_3d=(l == 0)), S=S)
        xchg_phase(nc, semstack, "xo%d" % l,
                   [(o_scr[i], og[i].rearrange("r p t -> (r p) t")) for i in range(4)])
        last = (l == depth - 1)
        moe_phase(nc, semstack, "m%d_" % l, dict(
            og=og, xres=(xT_own if l == 0 else x1_scr), w_out=w_out[l], ln1=ln1[l], ln2=ln2[l], rw=rw[l],
            rb=rb[l:l + 1, :], wgu=wgu[l], bguT=bguT[l], wdn=wdn[l], bdn=bdn[l], ident=ident, smask=smask,
            y_f32=(yT if last else x1_scr), y_b16=(None if last else xb_scr)), T=T, NE=NE, TB=TB)
        if not last:
            xchg_phase(nc, semstack, "xx%d" % l,
                       [(xb_scr[i], xg[i].rearrange("r j p t -> (r j p) t")) for i in range(4)])
    semstack.close()
    return nc


def _relayout_gu(w):
    L, E = w.shape[0], w.shape[1]
    v = w.reshape(L, E, 8, 128, 16, 128)
    order = [j for fc in range(8) for j in (fc, 8 + fc)]
    out = np.empty((L, E, 16, 128, 8, 128), np.float32)
    for ui, j in enumerate(order):
        out[:, :, ui] = v[:, :, :, :, j, :].transpose(0, 1, 3, 2, 4)
    return out.reshape(L, E, 16, 128, 1024)


def _relayout_dn(w):
    L, E = w.shape[0], w.shape[1]
    v = w.reshape(L, E, 8, 128, 8, 128)
    return np.ascontiguousarray(v.transpose(0, 1, 4, 3, 2, 5)).reshape(L, E, 8, 128, 1024)


def _wout_perm():
    rows = []
    for i in range(4):
        for r in range(2):
            st = [(2 * r) * 128, (2 * r + 1) * 128, 512 + (4 * r) * 64, 512 + (4 * r + 2) * 64][i]
            rows += list(range(st, st + 128))
    return rows


def make_in_maps(x, w_in, w_out, lambda_q1, lambda_k1, lambda_q2, lambda_k2, subln_g, t5_table, na_rpb,
                 ln1_g, ln1_b, router_w, router_b, w_gate_up, b_gate_up, w_down, b_down, ln2_g, ln2_b):
    f = np.float32
    A = lambda a: np.asarray(a, dtype=f)
    x = A(x)
    B_, S_, _ = x.shape
    depth = w_in.shape[0]
    NE = router_w.shape[-1]
    T = S_ // 2
    w_in = A(w_in)
    perm = _wout_perm()
    common = dict(
        w_out=np.ascontiguousarray(A(w_out)[:, perm, :]),
        ln1=np.stack([_pack_gb(A(ln1_g)[l], A(ln1_b)[l]) for l in range(depth)]),
        ln2=np.stack([_pack_gb(A(ln2_g)[l], A(ln2_b)[l]) for l in range(depth)]),
        rw=A(router_w), rb=A(router_b), wgu=_relayout_gu(A(w_gate_up)),
        bguT=np.stack([np.ascontiguousarray(A(b_gate_up)[l].reshape(NE, 16, 128).transpose(2, 0, 1).reshape(128, NE * 16))
                       for l in range(depth)]),
        wdn=_relayout_dn(A(w_down)), bdn=A(b_down), ident=np.eye(128, dtype=f))
    in_maps = []
    xTs = [np.ascontiguousarray(x[bi].T) for bi in range(B_)]
    for c in range(2 * B_):
        bi, r = c // 2, c % 2
        per = [prep_attn_inputs(x[bi][:8], w_in[l], r, l, A(lambda_q1)[l], A(lambda_k1)[l], A(lambda_q2)[l],
                                A(lambda_k2)[l], A(subln_g)[l], A(t5_table), A(na_rpb)[l], S=S_) for l in range(depth)]
        m = dict(common)
        m["xT_full"] = xTs[bi]
        m["xT_own"] = np.ascontiguousarray(xTs[bi][:, r * T:(r + 1) * T])
        sm = np.zeros((128, 2), f); sm[:, r] = 1.0
        m["smask"] = sm
        m["w_sel"] = np.stack([p["w_sel"] for p in per])
        m["t5c"] = per[0]["t5c"]; m["t5b"] = per[0]["t5b"]
        m["lamv"] = np.concatenate([p["lamv"] for p in per], axis=0)
        m["cst"] = np.stack([p["cst"] for p in per]); m["gsub"] = np.stack([p["gsub"] for p in per])
        m["nab"] = np.stack([p["nab"] for p in per])
        in_maps.append(m)
    return in_maps, (B_, S_, T)


def kernel(**inputs):
    in_maps, (B_, S_, T) = make_in_maps(**inputs)
    NE = np.asarray(inputs["router_w"]).shape[-1]
    depth = np.asarray(inputs["w_in"]).shape[0]
    nc = build_fused(S=S_, NE=NE, depth=depth)
    res = run_bass_kernel_spmd(nc, in_maps, core_ids=list(range(2 * B_)))
    out = np.empty((B_, S_, D), np.float32)
    for c in range(2 * B_):
        bi, r = c // 2, c % 2
        out[bi, r * T:(r + 1) * T, :] = res.results[c]["yT"].T
    return out
```

```python
import contextlib
import numpy as np
import concourse.bass as bass
import concourse.mybir as mybir
from concourse.bass_utils import run_bass_kernel_spmd

F32 = mybir.dt.float32
BF16 = mybir.dt.bfloat16
AF = mybir.ActivationFunctionType
ALU = mybir.AluOpType
AX = mybir.AxisListType

ENGS = ("pe", "act", "dve", "pool", "sp")


class Op:
    __slots__ = ("eng", "fn", "deps", "needed", "val", "ctr", "step")

    def __init__(self, eng, fn, ctr, step):
        self.eng = eng
        self.fn = fn
        self.deps = []
        self.needed = False
        self.val = 0
        self.ctr = ctr
        self.step = step


class Builder:
    def __init__(self, nc, semstack=None, tag=""):
        self.nc = nc
        self.semstack = semstack
        self.tag = tag
        self.ops = {e: [] for e in ENGS}
        self.lastw = {}
        self.readers = {}
        self.ctr_ops = {}
        self.stack = contextlib.ExitStack()
        self.dma_slots = set()

    def sbuf(self, name, shape, dt):
        return self.stack.enter_context(self.nc.sbuf_tensor(self.tag + name, list(shape), dt))

    def psum(self, name, shape, dt=F32):
        return self.stack.enter_context(self.nc.psum_tensor(self.tag + name, list(shape), dt))

    def _record(self, op, reads, writes):
        e = op.eng
        deps = []
        for k in reads:
            w = self.lastw.get(k)
            if w is not None:
                deps.append(w)
        for k in writes:
            w = self.lastw.get(k)
            if w is not None:
                deps.append(w)
            for r in self.readers.get(k, ()):
                if r.eng == e and r.step == 1 and op.step == 1:
                    continue
                deps.append(r)
        for d in deps:
            if d is op:
                continue
            if e == "pe" and d.eng == "pe" and d.step == 1 and op.step == 1:
                continue
            d.needed = True
            if d.step == 16:
                op.deps.append((d.ctr, 16 * len(self.ctr_ops[d.ctr])))
            else:
                op.deps.append(d)
        for k in reads:
            self.readers.setdefault(k, []).append(op)
        for k in writes:
            self.lastw[k] = op
            self.readers[k] = []
        self.ops[e].append(op)
        self.ctr_ops.setdefault(op.ctr, []).append(op)
        return op

    def op(self, eng, fn, reads=(), writes=()):
        return self._record(Op(eng, fn, eng, 1), reads, writes)

    def dma(self, eng, slot, fn, reads=(), writes=()):
        self.dma_slots.add(slot)
        o = Op(eng, fn, "dma:" + slot, 16)
        o.needed = True
        return self._record(o, reads, writes)

    def emit(self, final_waits=()):
        nc = self.nc
        ctrs = list(self.ctr_ops.keys())
        sems = {}
        for c in ctrs:
            sems[c] = (self.semstack or self.stack).enter_context(
                nc.semaphore("s_" + self.tag + c.replace(":", "_")))
        for c, ops in self.ctr_ops.items():
            v = 0
            for o in ops:
                if o.needed:
                    v += o.step
                    o.val = v
                else:
                    o.val = v
        engmap = {"pe": "tensor", "act": "scalar", "dve": "vector", "pool": "gpsimd", "sp": "sync"}
        fin = {}
        for o in final_waits:
            fin[o.ctr] = max(fin.get(o.ctr, 0), o.val)
        self.nwaits = 0
        with nc.Block() as block:
            for e in ENGS:
                ops = self.ops[e]
                last = (e == "sp")
                if not ops and not last:
                    continue

                def body(eng, ops=ops, e=e, last=last):
                    known = {}
                    for o in ops:
                        need = {}
                        for d in o.deps:
                            c, v = d if isinstance(d, tuple) else (d.ctr, d.val)
                            if v > need.get(c, 0):
                                need[c] = v
                        for c, v in need.items():
                            if known.get(c, 0) < v:
                                eng.wait_ge(sems[c], v)
                                known[c] = v
                                self.nwaits += 1
                        ins = o.fn(eng)
                        if o.needed:
                            ins.then_inc(sems[o.ctr], o.step)
                    if last:
                        for c, v in fin.items():
                            eng.wait_ge(sems[c], v)

                getattr(block, engmap[e])(body)

    def close(self):
        self.stack.close()


D = 1024
FF = 1024
KC = 8
SBK = 512
ALPHA = (2.0 * 2) ** 0.25
LN_EPS = 1e-5


def _ln_feature_major(b, P, zt, src_keys, ones_f, ps_a, ps_b, tmp, gb, out_fn, tag):
    sq, mean, var, rstd = tmp["a"], tmp["b"], tmp["c"], tmp["d"]
    for fc in range(KC):
        b.op("pe", lambda e, fc=fc: e.matmul(ps_a[:, :], ones_f[:, :], zt[:, fc, :],
                                            start=(fc == 0), stop=(fc == KC - 1)),
             reads=[("z", fc)], writes=[ps_a.name] if fc in (0, KC - 1) else [])
    for fc in range(KC):
        b.op("act", lambda e, fc=fc: e.activation(out=sq[:, :], in_=zt[:, fc, :], func=AF.Square),
             reads=[("z", fc)], writes=[sq.name])
        b.op("pe", lambda e, fc=fc: e.matmul(ps_b[:, :], ones_f[:, :], sq[:, :],
                                            start=(fc == 0), stop=(fc == KC - 1)),
             reads=[sq.name], writes=[ps_b.name] if fc in (0, KC - 1) else [])
    b.op("dve", lambda e: e.tensor_scalar(out=mean[:, :], in0=ps_a[:, :], scalar1=1.0 / D, scalar2=None,
                                          op0=ALU.mult), reads=[ps_a.name], writes=[mean.name])
    b.op("dve", lambda e: e.tensor_tensor(out=var[:, :], in0=mean[:, :], in1=mean[:, :], op=ALU.mult),
         reads=[mean.name], writes=[var.name])
    b.op("dve", lambda e: e.scalar_tensor_tensor(out=var[:, :], in0=ps_b[:, :], scalar=1.0 / D, in1=var[:, :],
                                                 op0=ALU.mult, op1=ALU.subtract),
         reads=[ps_b.name, var.name], writes=[var.name])
    b.op("dve", lambda e: e.tensor_scalar(out=var[:, :], in0=var[:, :], scalar1=LN_EPS, scalar2=None,
                                          op0=ALU.add), reads=[var.name], writes=[var.name])
    b.op("act", lambda e: e.activation(out=rstd[:, :], in_=var[:, :], func=AF.Sqrt),
         reads=[var.name], writes=[rstd.name])
    b.op("dve", lambda e: e.reciprocal(out=rstd[:, :], in_=rstd[:, :]), reads=[rstd.name], writes=[rstd.name])
    for fc in range(KC):
        b.op("dve", lambda e, fc=fc: e.tensor_tensor(out=zt[:, fc, :], in0=zt[:, fc, :], in1=mean[:, :],
                                                     op=ALU.subtract),
             reads=[("z", fc), mean.name], writes=[("z", fc)])
        b.op("pool", lambda e, fc=fc: e.tensor_tensor(out=zt[:, fc, :], in0=zt[:, fc, :], in1=rstd[:, :],
                                                      op=ALU.mult),
             reads=[("z", fc), rstd.name], writes=[("z", fc)])
        b.op("dve", lambda e, fc=fc: e.tensor_scalar(out=zt[:, fc, :], in0=zt[:, fc, :],
                                                     scalar1=gb[:, fc:fc + 1], scalar2=gb[:, 8 + fc:9 + fc],
                                                     op0=ALU.mult, op1=ALU.add),
             reads=[("z", fc), gb.name], writes=[("z", fc)])
        out_fn(fc)


def moe_phase(nc, semstack, tag, io, T=4096, NE=32, TB=1024):
    NTB = T // TB
    NSB = TB // SBK

    og = io["og"]; xres = io["xres"]; w_out = io["w_out"]; ln1 = io["ln1"]; ln2 = io["ln2"]
    rw = io["rw"]; rb = io["rb"]; wgu = io["wgu"]; bguT = io["bguT"]; wdn = io["wdn"]; bdn = io["bdn"]
    ident = io["ident"]; smask = io["smask"]; y_f32 = io["y_f32"]; y_b16 = io.get("y_b16")

    b = Builder(nc, semstack, tag)
    NU = 12
    ring = [b.sbuf(f"ring{i}", [128, 1024], BF16) for i in range(NU)]
    wo = b.sbuf("wo", [128, KC, D], BF16)
    acc = b.sbuf("acc", [128, KC, TB], F32)
    x1b = b.sbuf("x1b", [128, KC, TB], BF16)
    actp = [[b.sbuf(f"act{p}_{i}", [128, KC, SBK], BF16) for i in range(2)] for p in range(2)]
    actb = actp[0]
    z = b.sbuf("z", [128, KC, SBK], F32)
    tg = [b.sbuf(f"tg{i}", [128, SBK], F32) for i in range(2)]
    ts = [b.sbuf(f"ts{i}", [128, SBK], F32) for i in range(2)]
    tu = [b.sbuf(f"tu{i}", [128, SBK], F32) for i in range(2)]
    cbt = [[b.sbuf(f"cb{p}_{i}", [128, SBK], F32) for i in range(2)] for p in range(2)]
    lnt = {"a": tg[0], "b": ts[0], "c": tu[0], "d": tg[1]}
    ln1_sb = b.sbuf("ln1_sb", [128, 16], F32); ln2_sb = b.sbuf("ln2_sb", [128, 16], F32)
    rw_sb = b.sbuf("rw_sb", [128, KC, NE], F32); rb_sb = b.sbuf("rb_sb", [128, NE], F32)
    bgu_sb = b.sbuf("bgu_sb", [128, NE * 16], F32)
    bdn_sb = b.sbuf("bdn_sb", [NE, D], F32)
    cand = actp[1][0]
    sm_sb = b.sbuf("sm_sb", [128, 2], F32)
    zb = [b.sbuf(f"zb{i}", [128, SBK], BF16) for i in range(2)]
    id_sb = b.sbuf("id_sb", [128, 128], F32)
    ones_f = b.sbuf("ones_f", [128, 128], F32)
    combT = b.sbuf("combT", [NE, TB], F32)
    lg = b.sbuf("lg", [128, NE], F32); ex = b.sbuf("ex", [128, NE], F32); msk = b.sbuf("msk", [128, NE], F32)
    mx8 = b.sbuf("mx8", [128, 8], F32); nmx = b.sbuf("nmx", [128, 1], F32); ssum = b.sbuf("ssum", [128, 1], F32)

    ps_g = [b.psum(f"ps_g{i}", [128, SBK]) for i in range(2)]
    ps_u = [b.psum(f"ps_u{i}", [128, SBK]) for i in range(2)]
    ps_d = [b.psum(f"ps_d{i}", [128, SBK]) for i in range(2)]
    ps_c = b.psum("ps_c", [128, SBK])
    ps_m = b.psum("ps_m", [128, SBK])

    def ld(eng, slot, dst, src, key):
        b.dma(eng, slot, lambda e: e.dma_start(out=dst, in_=src), writes=[key])

    ld("sp", "c0", ln1_sb[:, :], ln1[:, :], ln1_sb.name)
    ld("sp", "c0", ln2_sb[:, :], ln2[:, :], ln2_sb.name)
    ld("sp", "c0", rw_sb[:, :, :], rw.rearrange("(kc p) n -> p kc n", p=128), rw_sb.name)
    ld("sp", "c0", rb_sb[:, :], rb[0:1, :].broadcast_to([128, NE]), rb_sb.name)
    ld("sp", "c0", bgu_sb[:, :], bguT[:, :], bgu_sb.name)
    ld("sp", "c0", bdn_sb[:, :], bdn[:, :], bdn_sb.name)
    ld("sp", "c0", sm_sb[:, :], smask[:, :], sm_sb.name)
    ld("sp", "c0", id_sb[:, :], ident[:, :], id_sb.name)
    b.op("dve", lambda e: e.memset(ones_f[:, :], 1.0), writes=[ones_f.name])
    bgu3 = bgu_sb[:, :].rearrange("p (e c) -> p e c", c=16)
    b.op("dve", lambda e: e.tensor_scalar(out=bgu3[:, :, 8:16], in0=bgu3[:, :, 8:16], scalar1=1.0, scalar2=None,
                                          op0=ALU.add), reads=[bgu_sb.name], writes=[bgu_sb.name])

    units = []
    for tb_ in range(NTB):
        for e_ in range(NE):
            for fc_ in range(KC):
                units.append(wgu[e_, 2 * fc_]); units.append(wgu[e_, 2 * fc_ + 1])
                if e_ >= 1:
                    units.append(wdn[e_ - 1, fc_])
        for fo_ in range(KC):
            units.append(wdn[NE - 1, fo_])
    st = {"loaded": 0, "n": 0}

    def _ensure(upto):
        while st["loaded"] < min(upto, len(units)):
            i = st["loaded"]
            u = ring[i % NU]
            src = units[i]
            b.dma("pool", "ring%d" % (i % NU), lambda e, u=u, src=src: e.dma_start(out=u[:, :], in_=src),
                  writes=[u.name])
            st["loaded"] += 1

    def next_unit():
        _ensure(st["n"] + 1)
        u = ring[st["n"] % NU]
        st["n"] += 1
        return u[:, :].rearrange("p (k c) -> p k c", k=KC), u.name

    def unit_done():
        _ensure(st["n"] + NU)

    b.dma("pool", "wo", lambda e: e.dma_start(out=wo[:, :, :], in_=w_out.rearrange("(kc p) f -> p kc f", p=128)),
          writes=[wo.name])

    out_ops = []
    for tb in range(NTB):
        t0 = tb * TB
        _ensure(st["n"] + NU)
        for sb in range(NSB):
            c0 = t0 + sb * SBK
            ob = actb[sb % 2]
            b.dma("pool", "ob" + str(sb % 2), lambda e, ob=ob, c0=c0: e.dma_start(
                out=ob[:, :, :], in_=og[:, :, :, c0:c0 + SBK].rearrange("i r p t -> p (i r) t")),
                writes=[(ob.name, k_) for k_ in range(KC)])
            b.dma("pool", "cand", lambda e, c0=c0: e.dma_start(
                out=cand[:, :, :], in_=og[:, :, :, T + c0:T + c0 + SBK].rearrange("i r p t -> p (i r) t")),
                writes=[(cand.name, k_) for k_ in range(KC)])
            b.op("dve", lambda e, ob=ob: e.tensor_scalar(out=ob[:, :, :], in0=ob[:, :, :], scalar1=sm_sb[:, 0:1],
                                                         scalar2=None, op0=ALU.mult),
                 reads=[(ob.name, k_) for k_ in range(KC)] + [sm_sb.name], writes=[(ob.name, k_) for k_ in range(KC)])
            b.op("dve", lambda e, ob=ob: e.scalar_tensor_tensor(out=ob[:, :, :], in0=cand[:, :, :], scalar=sm_sb[:, 1:2],
                                                                in1=ob[:, :, :], op0=ALU.mult, op1=ALU.add),
                 reads=[(ob.name, k_) for k_ in range(KC)] + [(cand.name, k_) for k_ in range(KC)] + [sm_sb.name],
                 writes=[(ob.name, k_) for k_ in range(KC)])
            for fc in range(KC):
                b.dma("sp", "zin", lambda e, fc=fc, c0=c0: e.dma_start(
                    out=z[:, fc, :], in_=xres[fc * 128:(fc + 1) * 128, c0:c0 + SBK]), writes=[("z", fc)])
            for fc in range(KC):
                pp = ps_d[fc % 2]
                for kc in range(KC):
                    b.op("pe", lambda e, pp=pp, kc=kc, fc=fc, ob=ob, wo=wo: e.matmul(
                        pp[:, :], wo[:, kc, fc * 128:(fc + 1) * 128], ob[:, kc, :],
                        start=(kc == 0), stop=(kc == KC - 1)),
                        reads=[wo.name, (ob.name, kc)], writes=[pp.name] if kc in (0, KC - 1) else [])
                b.op("dve", lambda e, pp=pp, fc=fc: e.scalar_tensor_tensor(
                    out=z[:, fc, :], in0=z[:, fc, :], scalar=ALPHA, in1=pp[:, :], op0=ALU.mult, op1=ALU.add),
                    reads=[("z", fc), pp.name], writes=[("z", fc)])

            def after_ln1(fc, sb=sb):
                cs = slice(sb * SBK, (sb + 1) * SBK)
                b.op("act", lambda e: e.activation(out=x1b[:, fc, cs], in_=z[:, fc, :], func=AF.Copy),
                     reads=[("z", fc)], writes=[("x1b", fc, sb)])
                b.op("pool", lambda e: e.tensor_scalar(out=acc[:, fc, cs], in0=z[:, fc, :], scalar1=ALPHA,
                                                       scalar2=None, op0=ALU.mult),
                     reads=[("z", fc)], writes=[("acc", fc, sb)])

            _ln_feature_major(b, None, z, None, ones_f, ps_c, ps_m, lnt, ln1_sb, after_ln1, "ln1")

            for tc in range(SBK // 128):
                for kc in range(KC):
                    b.op("pe", lambda e, tc=tc, kc=kc: e.matmul(
                        ps_c[:, 0:NE], z[:, kc, tc * 128:(tc + 1) * 128], rw_sb[:, kc, :],
                        start=(kc == 0), stop=(kc == KC - 1)),
                        reads=[("z", kc), rw_sb.name], writes=[ps_c.name] if kc in (0, KC - 1) else [])
                b.op("dve", lambda e: e.tensor_tensor(out=lg[:, :], in0=ps_c[:, 0:NE], in1=rb_sb[:, :], op=ALU.add),
                     reads=[ps_c.name, rb_sb.name], writes=[lg.name])
                b.op("dve", lambda e: e.max(out=mx8[:, :], in_=lg[:, :]), reads=[lg.name], writes=[mx8.name])
                b.op("dve", lambda e: e.tensor_scalar(out=msk[:, :], in0=lg[:, :], scalar1=mx8[:, 3:4], scalar2=None,
                                                      op0=ALU.is_ge), reads=[lg.name, mx8.name], writes=[msk.name])
                b.op("dve", lambda e: e.tensor_scalar(out=nmx[:, :], in0=mx8[:, 0:1], scalar1=-1.0, scalar2=None,
                                                      op0=ALU.mult), reads=[mx8.name], writes=[nmx.name])
                b.op("act", lambda e: e.activation(out=ex[:, :], in_=lg[:, :], func=AF.Exp, bias=nmx[:, 0:1], scale=1.0),
                     reads=[lg.name, nmx.name], writes=[ex.name])
                b.op("dve", lambda e: e.tensor_tensor(out=ex[:, :], in0=ex[:, :], in1=msk[:, :], op=ALU.mult),
                     reads=[ex.name, msk.name], writes=[ex.name])
                b.op("dve", lambda e: e.reduce_sum(out=ssum[:, :], in_=ex[:, :], axis=AX.X),
                     reads=[ex.name], writes=[ssum.name])
                b.op("dve", lambda e: e.reciprocal(out=ssum[:, :], in_=ssum[:, :]), reads=[ssum.name], writes=[ssum.name])
                b.op("dve", lambda e: e.tensor_scalar(out=ex[:, :], in0=ex[:, :], scalar1=ssum[:, 0:1], scalar2=None,
                                                      op0=ALU.mult), reads=[ex.name, ssum.name], writes=[ex.name])
                b.op("pe", lambda e: e.transpose(out=ps_m[0:NE, 0:128], in_=ex[:, :], identity=id_sb[:, :]),
                     reads=[ex.name, id_sb.name], writes=[ps_m.name])
                cofs = sb * SBK + tc * 128
                b.op("act", lambda e, cofs=cofs: e.activation(out=combT[:, cofs:cofs + 128], in_=ps_m[0:NE, 0:128],
                                                              func=AF.Copy),
                     reads=[ps_m.name], writes=[("combT", sb, tc)])
            cs = slice(sb * SBK, (sb + 1) * SBK)
            for fo in range(KC):
                pp = ps_d[fo % 2]
                b.op("pe", lambda e, pp=pp, fo=fo, cs=cs: e.matmul(pp[:, :], bdn_sb[:, fo * 128:(fo + 1) * 128],
                                                                  combT[:, cs], start=True, stop=True),
                     reads=[bdn_sb.name] + [("combT", sb, tc) for tc in range(4)], writes=[pp.name])
                b.op("dve", lambda e, pp=pp, fo=fo, cs=cs: e.tensor_tensor(out=acc[:, fo, cs], in0=acc[:, fo, cs],
                                                                          in1=pp[:, :], op=ALU.add),
                     reads=[("acc", fo, sb), pp.name], writes=[("acc", fo, sb)])

        tile_q = []
        tcount = [0]

        def stage2(item):
            g_t, s_t, u_t, cb, ab, fc = item
            b.op("dve", lambda e: e.tensor_scalar(out=u_t[:, :], in0=u_t[:, :], scalar1=-6.0, scalar2=8.0,
                                                  op0=ALU.max, op1=ALU.min), reads=[u_t.name], writes=[u_t.name])
            b.op("dve", lambda e: e.tensor_tensor(out=u_t[:, :], in0=u_t[:, :], in1=cb[:, :], op=ALU.mult),
                 reads=[u_t.name, cb.name], writes=[u_t.name])
            b.op("dve", lambda e: e.tensor_tensor(out=g_t[:, :], in0=g_t[:, :], in1=s_t[:, :], op=ALU.mult),
                 reads=[g_t.name, s_t.name], writes=[g_t.name])
            b.op("dve", lambda e: e.tensor_tensor(out=ab[:, fc, :], in0=g_t[:, :], in1=u_t[:, :], op=ALU.mult),
                 reads=[g_t.name, u_t.name], writes=[(ab.name, fc)])

        def down(ex_d, fo):
            Dn, Dkey = next_unit()
            for sb in range(NSB):
                cs = slice(sb * SBK, (sb + 1) * SBK)
                ab = actp[ex_d % 2][sb]
                pd = ps_d[sb % 2]
                for fc in range(KC):
                    b.op("pe", lambda e, pd=pd, fc=fc, ab=ab, Dn=Dn: e.matmul(
                        pd[:, :], Dn[:, fc, :], ab[:, fc, :], start=(fc == 0), stop=(fc == KC - 1)),
                        reads=[Dkey, (ab.name, fc)], writes=[pd.name] if fc in (0, KC - 1) else [])
                b.op("dve", lambda e, pd=pd, fo=fo, cs=cs: e.tensor_tensor(out=acc[:, fo, cs], in0=acc[:, fo, cs],
                                                                          in1=pd[:, :], op=ALU.add),
                     reads=[("acc", fo, sb), pd.name], writes=[("acc", fo, sb)])
            unit_done()

        for ex_i in range(NE):
            par = ex_i % 2
            for sb in range(NSB):
                cs = slice(sb * SBK, (sb + 1) * SBK)
                cb = cbt[par][sb]
                b.op("pe", lambda e, cs=cs, ex_i=ex_i: e.matmul(ps_c[:, :], id_sb[0:NE, ex_i:ex_i + 1].broadcast_to([NE, 128]),
                                                               combT[:, cs], start=True, stop=True),
                     reads=[id_sb.name] + [("combT", sb, tc) for tc in range(4)], writes=[ps_c.name])
                b.op("act", lambda e, cb=cb: e.activation(out=cb[:, :], in_=ps_c[:, :], func=AF.Copy),
                     reads=[ps_c.name], writes=[cb.name])
            for fc in range(KC):
                G, Gkey = next_unit()
                U, Ukey = next_unit()
                for W, Wkey, pz in ((G, Gkey, ps_g), (U, Ukey, ps_u)):
                    for kc in range(KC):
                        for sb in range(NSB):
                            cs = slice(sb * SBK, (sb + 1) * SBK)
                            b.op("pe", lambda e, W=W, pz=pz, kc=kc, sb=sb, cs=cs: e.matmul(
                                pz[sb][:, :], W[:, kc, :], x1b[:, kc, cs], start=(kc == 0), stop=(kc == KC - 1)),
                                reads=[Wkey, ("x1b", kc, sb)], writes=[pz[sb].name] if kc in (0, KC - 1) else [])
                unit_done()
                bg = bgu_sb[:, ex_i * 16 + fc: ex_i * 16 + fc + 1]
                bu = bgu_sb[:, ex_i * 16 + 8 + fc: ex_i * 16 + 8 + fc + 1]
                for sb in range(NSB):
                    ti = tcount[0] % 2
                    tcount[0] += 1
                    g_t = tg[ti]; s_t = ts[ti]; u_t = tu[ti]
                    pg = ps_g[sb]; pu = ps_u[sb]
                    b.op("dve", lambda e, pg=pg, g_t=g_t, bg=bg: e.tensor_scalar(
                        out=g_t[:, :], in0=pg[:, :], scalar1=bg, scalar2=7.0, op0=ALU.add, op1=ALU.min),
                        reads=[pg.name, bgu_sb.name], writes=[g_t.name])
                    b.op("act", lambda e, g_t=g_t, s_t=s_t: e.activation(out=s_t[:, :], in_=g_t[:, :],
                                                                         func=AF.Sigmoid, scale=1.702),
                         reads=[g_t.name], writes=[s_t.name])
                    b.op("act", lambda e, pu=pu, u_t=u_t, bu=bu: e.activation(out=u_t[:, :], in_=pu[:, :],
                                                                             func=AF.Identity, bias=bu, scale=1.0),
                         reads=[pu.name, bgu_sb.name], writes=[u_t.name])
                    if tile_q:
                        stage2(tile_q.pop(0))
                    tile_q.append((g_t, s_t, u_t, cbt[par][sb], actp[par][sb], fc))
                if ex_i >= 1:
                    down(ex_i - 1, fc)
        while tile_q:
            stage2(tile_q.pop(0))
        for fo in range(KC):
            down(NE - 1, fo)

        for sb in range(NSB):
            cs = slice(sb * SBK, (sb + 1) * SBK)
            c0 = t0 + sb * SBK
            for fc in range(KC):
                b.op("act", lambda e, fc=fc, cs=cs: e.activation(out=z[:, fc, :], in_=acc[:, fc, cs], func=AF.Copy),
                     reads=[("acc", fc, sb)], writes=[("z", fc)])

            def after_ln2(fc, c0=c0):
                o = b.dma("sp", "yout", lambda e: e.dma_start(out=y_f32[fc * 128:(fc + 1) * 128, c0:c0 + SBK],
                                                              in_=z[:, fc, :]), reads=[("z", fc)])
                out_ops.append(o)
                if y_b16 is not None:
                    zb_ = zb[fc % 2]
                    b.op("act", lambda e: e.activation(out=zb_[:, :], in_=z[:, fc, :], func=AF.Copy),
                         reads=[("z", fc)], writes=[zb_.name])
                    o2 = b.dma("sp", "youtb%d" % (fc % 2), lambda e: e.dma_start(
                        out=y_b16[fc // 2, (fc % 2) * 128:(fc % 2 + 1) * 128, c0:c0 + SBK], in_=zb_[:, :]),
                        reads=[zb_.name])
                    out_ops.append(o2)

            _ln_feature_major(b, None, z, None, ones_f, ps_c, ps_m, lnt, ln2_sb, after_ln2, "ln2")

    b.emit(final_waits=out_ops)
    b.close()


def attn_phase(nc, semstack, tag, io, S=8192):
    NQB = None
    NTB = S // SBK
    NKB = S // 128
    NRP = S // 128
    if NQB is None:
        NQB = NTB

    w_sel = io["w_sel"]; t5c = io["t5c"]; t5b = io["t5b"]; lamv = io["lamv"]; cst = io["cst"]
    gsub = io["gsub"]; nab = io["nab"]; o_scr = io["o_scr"]; x_src = io["x_src"]

    b = Builder(nc, semstack, tag)
    BQ = [b.sbuf(f"BQ{i}", [128, S], BF16) for i in range(2)]
    BK = [b.sbuf(f"BK{i}", [128, S], BF16) for i in range(2)]
    BV = b.sbuf("BV", [128, NKB, 256], BF16)
    wsb = b.sbuf("wsb", [128, KC, 768], BF16)
    xb = [b.sbuf(f"xb{i}", [128, KC, SBK], BF16) for i in range(2)]
    t5b_sb = b.sbuf("t5b_sb", [128, 12, SBK], F32)
    nab_sb = [b.sbuf(f"nab{i}", [128, 640], F32) for i in range(2)]
    t5c_sb = b.sbuf("t5c_sb", [128, 4], F32)
    lam_sb = b.sbuf("lam_sb", [128, 256], F32); cst_sb = b.sbuf("cst_sb", [128, 4], F32)
    gcol = b.sbuf("gcol", [128, 1], F32); nlam = b.sbuf("nlam", [128, 1], F32)
    e12 = b.sbuf("e12", [128, 2], F32); lprod = b.sbuf("lprod", [128, 128], F32)
    ones_b = b.sbuf("ones_b", [128, 128], BF16); ones_f = b.sbuf("ones_f", [128, 128], F32)
    tA = b.sbuf("tA", [128, SBK], F32); tB = b.sbuf("tB", [128, SBK], F32)
    tAo = [b.sbuf(f"tAo{i}", [128, SBK], BF16) for i in range(2)]
    nsb = [b.sbuf(f"nsb{i}", [128, 640], F32) for i in range(2)]
    nP = [b.sbuf(f"nP{i}", [128, 640], BF16) for i in range(2)]
    nr = b.sbuf("nr", [64, 128], F32)
    nout = [b.sbuf(f"nout{i}", [64, SBK], BF16) for i in range(2)]

    class _Bank:
        def __init__(self, t, j):
            self.t = t; self.j = j; self.name = t.name + "_b%d" % j

        def __getitem__(self, idx):
            return self.t[:, self.j, :][idx]

    PP = [b.psum(f"PP{i}", [128, 2, SBK]) for i in range(4)]
    psb = [_Bank(PP[i // 2], i % 2) for i in range(8)]
    Pt = [[b.sbuf(f"P{m}_{i}", [128, SBK], BF16) for i in range(2)] for m in range(2)]
    tsp = [b.sbuf(f"tsp{i}", [128, SBK], F32) for i in range(2)]

    def ld(eng, slot, dst, src, key):
        b.dma(eng, slot, lambda e: e.dma_start(out=dst, in_=src), writes=[key])

    ld("sp", "c0", t5c_sb[:, :], t5c[:, :], t5c_sb.name)
    ld("sp", "c0", lam_sb[:, :], lamv[0:1, :].broadcast_to([128, 256]), lam_sb.name)
    ld("sp", "c0", cst_sb[:, :], cst[:, :], cst_sb.name)
    ld("sp", "c0", gcol[:, :], gsub[:, :], gcol.name)
    for h in range(2):
        for v in range(6):
            ld("sp", "c0", t5b_sb[:, h * 6 + v, :], t5b[h, v, :, :], t5b_sb.name)
    b.op("dve", lambda e: e.memset(ones_f[:, :], 1.0), writes=[ones_f.name])
    b.op("dve", lambda e: e.memset(ones_b[:, :], 1.0), writes=[ones_b.name])
    b.op("dve", lambda e: e.tensor_tensor(out=lprod[:, 0:64], in0=lam_sb[:, 0:64], in1=lam_sb[:, 64:128], op=ALU.mult),
         reads=[lam_sb.name], writes=[lprod.name])
    b.op("dve", lambda e: e.tensor_tensor(out=lprod[:, 64:128], in0=lam_sb[:, 128:192], in1=lam_sb[:, 192:256], op=ALU.mult),
         reads=[lam_sb.name, lprod.name], writes=[lprod.name])
    b.op("dve", lambda e: e.reduce_sum(out=e12[:, 0:1], in_=lprod[:, 0:64], axis=AX.X), reads=[lprod.name], writes=[e12.name])
    b.op("dve", lambda e: e.reduce_sum(out=e12[:, 1:2], in_=lprod[:, 64:128], axis=AX.X), reads=[lprod.name, e12.name], writes=[e12.name])
    b.op("act", lambda e: e.activation(out=e12[:, :], in_=e12[:, :], func=AF.Exp), reads=[e12.name], writes=[e12.name])
    b.op("dve", lambda e: e.tensor_tensor(out=nlam[:, :], in0=e12[:, 1:2], in1=e12[:, 0:1], op=ALU.subtract),
         reads=[e12.name], writes=[nlam.name])
    b.op("dve", lambda e: e.tensor_tensor(out=nlam[:, :], in0=nlam[:, :], in1=cst_sb[:, 0:1], op=ALU.subtract),
         reads=[nlam.name, cst_sb.name], writes=[nlam.name])
    b.op("dve", lambda e: e.tensor_tensor(out=gcol[:, :], in0=gcol[:, :], in1=cst_sb[:, 1:2], op=ALU.mult),
         reads=[gcol.name, cst_sb.name], writes=[gcol.name])

    out_ops = []

    def in_proj(col0):
        b.dma("pool", "wsb", lambda e: e.dma_start(
            out=wsb[:, :, :], in_=w_sel[:, col0:col0 + 768].rearrange("(kc p) f -> p kc f", p=128)),
            writes=[wsb.name])
        for tb in range(NTB):
            x_ = xb[tb % 2]
            if io["x_3d"]:
                b.dma("pool", "xb%d" % (tb % 2), lambda e, x_=x_, tb=tb: e.dma_start(
                    out=x_[:, :, :], in_=x_src(tb)), writes=[x_.name])
            else:
                for j in range(2):
                    b.dma("pool", "xb%d" % (tb % 2), lambda e, x_=x_, tb=tb, j=j: e.dma_start(
                        out=x_[:, :, :].rearrange("p (i j) t -> p i j t", i=4)[:, :, j, :],
                        in_=x_src(tb, j)), writes=[x_.name])
            for oc in range(4):
                pp = psb[oc % 2]
                for kc in range(KC):
                    b.op("pe", lambda e, pp=pp, kc=kc, oc=oc, x_=x_: e.matmul(
                        pp[:, :], wsb[:, kc, oc * 128:(oc + 1) * 128], x_[:, kc, :],
                        start=(kc == 0), stop=(kc == KC - 1)),
                        reads=[wsb.name, x_.name], writes=[pp.name] if kc in (0, KC - 1) else [])
                dst = (BQ if oc < 2 else BK)[oc % 2]
                sc = 0.125 if oc < 2 else 1.0
                eng = "act" if oc % 2 == 0 else "dve"
                if eng == "act":
                    b.op("act", lambda e, pp=pp, dst=dst, tb=tb, sc=sc: e.activation(
                        out=dst[:, tb * SBK:(tb + 1) * SBK], in_=pp[:, :], func=AF.Copy, scale=sc),
                        reads=[pp.name], writes=[(dst.name, tb)])
                else:
                    b.op("dve", lambda e, pp=pp, dst=dst, tb=tb, sc=sc: e.tensor_scalar(
                        out=dst[:, tb * SBK:(tb + 1) * SBK], in0=pp[:, :], scalar1=sc, scalar2=None, op0=ALU.mult),
                        reads=[pp.name], writes=[(dst.name, tb)])
            for tc in range(4):
                pp = psb[2 + tc % 2]
                for kc in range(KC):
                    b.op("pe", lambda e, pp=pp, kc=kc, tc=tc, x_=x_: e.matmul(
                        pp[:, 0:256], x_[:, kc, tc * 128:(tc + 1) * 128], wsb[:, kc, 512:768],
                        start=(kc == 0), stop=(kc == KC - 1)),
                        reads=[wsb.name, x_.name], writes=[pp.name] if kc in (0, KC - 1) else [])
                ch = tb * 4 + tc
                if tc % 2 == 0:
                    b.op("act", lambda e, pp=pp, ch=ch: e.activation(out=BV[:, ch, :], in_=pp[:, 0:256], func=AF.Copy),
                         reads=[pp.name], writes=[("BV", ch)])
                else:
                    b.op("dve", lambda e, pp=pp, ch=ch: e.tensor_copy(out=BV[:, ch, :], in_=pp[:, 0:256]),
                         reads=[pp.name], writes=[("BV", ch)])

    in_proj(0)
    for h in range(2):
        for qb in range(NQB):
            qs = slice(qb * SBK, (qb + 1) * SBK)
            po = [psb[4], psb[5]]; pd = [psb[6], psb[7]]

            def qk(kb, h=h, qb=qb, qs=qs):
                for m in range(2):
                    pp = psb[(kb % 2) * 2 + m]
                    b.op("pe", lambda e, pp=pp, m=m: e.matmul(
                        pp[:, :], BK[h][m * 64:(m + 1) * 64, kb * 128:(kb + 1) * 128], BQ[h][m * 64:(m + 1) * 64, qs],
                        start=True, stop=True),
                        reads=[(BK[h].name, kb // 4), (BQ[h].name, qb)], writes=[pp.name])

            def expav(kb, h=h, qb=qb):
                rel = kb - 4 * qb
                for m in range(2):
                    pp = psb[(kb % 2) * 2 + m]
                    P = Pt[m][kb % 2]
                    if -1 <= rel <= 4:
                        tt = tsp[m]
                        bt = t5b_sb[:, h * 6 + rel + 1, :]
                        b.op("dve", lambda e, pp=pp, tt=tt, bt=bt: e.tensor_tensor(out=tt[:, :], in0=pp[:, :], in1=bt, op=ALU.add),
                             reads=[pp.name, t5b_sb.name], writes=[tt.name])
                        b.op("act", lambda e, tt=tt, P=P: e.activation(out=P[:, :], in_=tt[:, :], func=AF.Exp),
                             reads=[tt.name], writes=[P.name])
                    else:
                        col = h * 2 + (0 if rel < 0 else 1)
                        b.op("act", lambda e, pp=pp, P=P, col=col: e.activation(
                            out=P[:, :], in_=pp[:, :], func=AF.Exp, bias=t5c_sb[:, col:col + 1], scale=1.0),
                            reads=[pp.name, t5c_sb.name], writes=[P.name])
                for m in range(2):
                    P = Pt[m][kb % 2]
                    b.op("pe", lambda e, P=P, m=m: e.matmul(
                        po[m][:, :], BV[:, kb, h * 128:(h + 1) * 128], P[:, :], start=(kb == 0), stop=(kb == NKB - 1)),
                        reads=[("BV", kb), P.name], writes=[po[m].name] if kb in (0, NKB - 1) else [])
                    b.op("pe", lambda e, P=P, m=m: e.matmul(
                        pd[m][:, :], ones_b[:, :], P[:, :], start=(kb == 0), stop=(kb == NKB - 1)),
                        reads=[ones_b.name, P.name], writes=[pd[m].name] if kb in (0, NKB - 1) else [])

            qk(0)
            for kb in range(NKB):
                if kb + 1 < NKB:
                    qk(kb + 1)
                expav(kb)
            b.op("dve", lambda e: e.reciprocal(out=tA[:, :], in_=pd[0][:, :]), reads=[pd[0].name], writes=[tA.name])
            b.op("dve", lambda e: e.tensor_tensor(out=tA[:, :], in0=po[0][:, :], in1=tA[:, :], op=ALU.mult),
                 reads=[po[0].name, tA.name], writes=[tA.name])
            b.op("dve", lambda e: e.reciprocal(out=tB[:, :], in_=pd[1][:, :]), reads=[pd[1].name], writes=[tB.name])
            b.op("dve", lambda e: e.tensor_tensor(out=tB[:, :], in0=po[1][:, :], in1=tB[:, :], op=ALU.mult),
                 reads=[po[1].name, tB.name], writes=[tB.name])
            b.op("dve", lambda e: e.scalar_tensor_tensor(out=tA[:, :], in0=tB[:, :], scalar=nlam[:, 0:1], in1=tA[:, :],
                                                         op0=ALU.mult, op1=ALU.add),
                 reads=[tA.name, tB.name, nlam.name], writes=[tA.name])
            b.op("act", lambda e: e.activation(out=tB[:, :], in_=tA[:, :], func=AF.Square), reads=[tA.name], writes=[tB.name])
            pn = psb[0]
            b.op("pe", lambda e: e.matmul(pn[:, :], ones_f[:, :], tB[:, :], start=True, stop=True),
                 reads=[ones_f.name, tB.name], writes=[pn.name])
            b.op("dve", lambda e: e.tensor_scalar(out=tB[:, :], in0=pn[:, :], scalar1=1.0 / 128, scalar2=1e-5,
                                                  op0=ALU.mult, op1=ALU.add), reads=[pn.name], writes=[tB.name])
            b.op("act", lambda e: e.activation(out=tB[:, :], in_=tB[:, :], func=AF.Sqrt), reads=[tB.name], writes=[tB.name])
            b.op("dve", lambda e: e.reciprocal(out=tB[:, :], in_=tB[:, :]), reads=[tB.name], writes=[tB.name])
            b.op("dve", lambda e: e.tensor_tensor(out=tA[:, :], in0=tA[:, :], in1=tB[:, :], op=ALU.mult),
                 reads=[tA.name, tB.name], writes=[tA.name])
            to_ = tAo[qb % 2]
            b.op("dve", lambda e, to_=to_: e.tensor_scalar(out=to_[:, :], in0=tA[:, :], scalar1=gcol[:, 0:1], scalar2=None, op0=ALU.mult),
                 reads=[tA.name, gcol.name], writes=[to_.name])
            o = b.dma("sp", "oda%d" % (qb % 2), lambda e, h=h, qs=qs, to_=to_: e.dma_start(out=o_scr[h, :, qs], in_=to_[:, :]),
                      reads=[to_.name])
            out_ops.append(o)

    in_proj(768)
    NRPQ = NRP if NQB == NTB else NQB * 4
    na_units = []
    for n in range(4):
        for rp in range(NRPQ):
            na_units.append((n, rp))

    def na_qk(ui):
        n, rp = na_units[ui]
        p = n // 2
        pb = (n % 2) * 64
        cs = min(max(rp - 2, 0), NRP - 5)
        if rp == 0:
            v = 1
        elif rp == 1:
            v = 2
        elif rp == NRP - 2:
            v = 3
        elif rp == NRP - 1:
            v = 4
        else:
            v = 0
        i2 = ui % 2
        nb_ = nab_sb[i2]
        b.dma("sp", "nab%d" % i2, lambda e, nb_=nb_, n=n, v=v: e.dma_start(out=nb_[:, :], in_=nab[n, v, :, :]),
              writes=[nb_.name])
        psA = psb[i2 * 2]; psB = psb[i2 * 2 + 1]
        for c in range(5):
            dst = psA[:, c * 128:(c + 1) * 128] if c < 4 else psB[:, 0:128]
            b.op("pe", lambda e, dst=dst, c=c, p=p, pb=pb, cs=cs, rp=rp: e.matmul(
                dst, BK[p][pb:pb + 64, (cs + c) * 128:(cs + c + 1) * 128], BQ[p][pb:pb + 64, rp * 128:(rp + 1) * 128],
                start=True, stop=True),
                reads=[(BK[p].name, (cs + c) // 4), (BQ[p].name, rp // 4)],
                writes=[psA.name if c < 4 else psB.name])

    def na_rest(ui):
        n, rp = na_units[ui]
        cs = min(max(rp - 2, 0), NRP - 5)
        i2 = ui % 2
        nb_ = nab_sb[i2]
        psA = psb[i2 * 2]; psB = psb[i2 * 2 + 1]
        ppo = psb[4 + i2]; ppd = psb[6 + i2]
        sb_ = nsb[i2]; P = nP[i2]
        b.op("dve", lambda e: e.tensor_tensor(out=sb_[:, 0:512], in0=psA[:, :], in1=nb_[:, 0:512], op=ALU.add),
             reads=[psA.name, nb_.name], writes=[(sb_.name, 0)])
        b.op("dve", lambda e: e.tensor_tensor(out=sb_[:, 512:640], in0=psB[:, 0:128], in1=nb_[:, 512:640], op=ALU.add),
             reads=[psB.name, nb_.name], writes=[(sb_.name, 1)])
        b.op("act", lambda e: e.activation(out=P[:, :], in_=sb_[:, :], func=AF.Exp),
             reads=[(sb_.name, 0), (sb_.name, 1)], writes=[P.name])
        for c in range(5):
            b.op("pe", lambda e, c=c: e.matmul(
                ppo[0:64, 0:128], BV[:, cs + c, n * 64:(n + 1) * 64], P[:, c * 128:(c + 1) * 128],
                start=(c == 0), stop=(c == 4)),
                reads=[("BV", cs + c), P.name], writes=[ppo.name] if c in (0, 4) else [])
        for c in range(5):
            b.op("pe", lambda e, c=c: e.matmul(
                ppd[0:64, 0:128], ones_b[:, 0:64], P[:, c * 128:(c + 1) * 128], start=(c == 0), stop=(c == 4)),
                reads=[ones_b.name, P.name], writes=[ppd.name] if c in (0, 4) else [])
        no = nout[(rp // 4) % 2]
        b.op("dve", lambda e: e.reciprocal(out=nr[:, :], in_=ppd[0:64, 0:128]), reads=[ppd.name], writes=[nr.name])
        b.op("dve", lambda e: e.tensor_tensor(
            out=no[:, (rp % 4) * 128:(rp % 4 + 1) * 128], in0=ppo[0:64, 0:128], in1=nr[:, :], op=ALU.mult),
            reads=[ppo.name, nr.name], writes=[(no.name, rp % 4)])
        if rp % 4 == 3:
            r0 = (rp // 4) * SBK
            o = b.dma("sp", "ona%d" % ((rp // 4) % 2), lambda e: e.dma_start(
                out=o_scr[2 + n // 2, (n % 2) * 64:(n % 2 + 1) * 64, r0:r0 + SBK], in_=no[:, :]),
                reads=[(no.name, i) for i in range(4)])
            out_ops.append(o)

    na_qk(0)
    for ui in range(len(na_units)):
        if ui + 1 < len(na_units):
            na_qk(ui + 1)
        na_rest(ui)

    b.emit(final_waits=out_ops)
    b.close()


import math as _math

HEAD_DIM = 64
GRID_W = 64
NEG = -30000.0


def _t5_bucket_np(rel):
    rel = np.asarray(rel, np.int32)
    half = 16
    max_exact = 8
    ret = (rel > 0).astype(np.int32) * half
    n = np.abs(rel)
    n_f = np.maximum(n, 1).astype(np.float32)
    large = max_exact + (np.log(n_f / np.float32(max_exact)) / np.float32(_math.log(128 / max_exact))
                         * np.float32(half - max_exact)).astype(np.int32)
    large = np.minimum(large, half - 1)
    return ret + np.where(n < max_exact, n, large)


def _t5_tiles_idx():
    p = np.arange(128)[:, None]
    j = np.arange(SBK)[None, :]
    return np.stack([_t5_bucket_np((v - 1) * 128 + p - j) for v in range(6)])


def _na_tiles_idx(rows):
    nrp = rows // 2
    reps = {0: 2 if nrp > 4 else None, 1: 0, 2: 1, 3: nrp - 2, 4: nrp - 1}
    col_start = np.clip(np.arange(GRID_W) - 8, 0, GRID_W - 16)
    ro = np.zeros((5, 128, 640), np.int64); co = np.zeros((5, 128, 640), np.int64)
    va = np.zeros((5, 128, 640), bool)
    kic = np.arange(128)
    q = np.arange(128)
    for v, rp in reps.items():
        if rp is None:
            continue
        cs = min(max(rp - 2, 0), nrp - 5)
        for c in range(5):
            krow = 2 * (cs + c) + kic // 64
            kcol = kic % 64
            r = 2 * rp + q // 64
            w = q % 64
            rs = np.clip(r - 4, 0, rows - 8)
            valid = ((krow[:, None] >= rs[None, :]) & (krow[:, None] < rs[None, :] + 8) &
                     (kcol[:, None] >= col_start[w][None, :]) & (kcol[:, None] < col_start[w][None, :] + 16))
            roff = krow[:, None] - r[None, :] + 7
            coff = kcol[:, None] - w[None, :] + 15
            sl = slice(c * 128, (c + 1) * 128)
            va[v, :, sl] = valid
            ro[v, :, sl] = np.where(valid, roff, 0)
            co[v, :, sl] = np.where(valid, coff, 0)
    return ro, co, va


def prep_attn_inputs(x_b, w_in_l, hh, layer, lq1, lk1, lq2, lk2, subln_g, t5_table, na_rpb_l, S=None):
    f = np.float32
    S = x_b.shape[0] if S is None else S
    dah = [2 * hh, 2 * hh + 1]
    nah = [4 * hh + i for i in range(4)]
    o_q1, o_q2, o_k1, o_k2, o_va, o_qn, o_kn, o_vn = 0, 256, 512, 768, 1024, 1536, 2048, 2560
    cols = []
    for h in dah:
        cols += list(range(o_q1 + 64 * h, o_q1 + 64 * h + 64)) + list(range(o_q2 + 64 * h, o_q2 + 64 * h + 64))
    for h in dah:
        cols += list(range(o_k1 + 64 * h, o_k1 + 64 * h + 64)) + list(range(o_k2 + 64 * h, o_k2 + 64 * h + 64))
    for h in dah:
        cols += list(range(o_va + 128 * h, o_va + 128 * h + 128))
    for base in (o_qn, o_kn, o_vn):
        for n in nah:
            cols += list(range(base + 64 * n, base + 64 * n + 64))
    w_sel = np.ascontiguousarray(w_in_l[:, cols]).astype(f)
    t5c = np.empty((128, 4), f)
    for i, h in enumerate(dah):
        t5c[:, 2 * i] = t5_table[15, h]
        t5c[:, 2 * i + 1] = t5_table[31, h]
    tidx = _t5_tiles_idx()
    t5b = np.stack([t5_table[:, h][tidx] for h in dah]).astype(f)
    lamv = np.concatenate([lq1, lk1, lq2, lk2]).astype(f)[None, :]
    lam_init = 0.8 - 0.6 * _math.exp(-0.3 * layer)
    cst = np.zeros((128, 4), f); cst[:, 0] = lam_init; cst[:, 1] = 1.0 - lam_init
    ro, co, va = _na_tiles_idx(S // GRID_W)
    nab = np.stack([np.where(va, na_rpb_l[n][ro, co], f(NEG)) for n in nah]).astype(f)
    return dict(w_sel=w_sel, t5c=t5c, t5b=t5b, lamv=lamv, cst=cst,
                gsub=np.ascontiguousarray(subln_g.astype(f)[:, None]), nab=nab)


def _pack_gb(g, bb):
    return np.ascontiguousarray(np.concatenate([g.reshape(8, 128).T, bb.reshape(8, 128).T], axis=1)).astype(np.float32)


PAIRS = [[0, 1], [2, 3], [4, 5], [6, 7]]


def xchg_phase(nc, semstack, tag, pairs):
    sem = semstack.enter_context(nc.semaphore("cc_" + tag))
    with nc.Block() as block:
        @block.gpsimd
        def _(g):
            for src, dst in pairs:
                g.collective_compute("AllGather", ALU.bypass, replica_groups=PAIRS,
                                     ins=[src], outs=[dst]).then_inc(sem)
            g.wait_ge(sem, len(pairs))


def build_fused(S=8192, NE=32, depth=2, TB=1024):
    nc = bass.Bass("TRN2", target_bir_lowering=False)
    T = S // 2

    def din(name, shape):
        return nc.dram_tensor(name, list(shape), F32, kind="ExternalInput").ap()

    xT_full = din("xT_full", [D, S]); xT_own = din("xT_own", [D, T]); smask = din("smask", [128, 2])
    w_sel = din("w_sel", [depth, D, 1536]); t5c = din("t5c", [128, 4]); t5b = din("t5b", [2, 6, 128, SBK])
    lamv = din("lamv", [depth, 256]); cst = din("cst", [depth, 128, 4]); gsub = din("gsub", [depth, 128, 1])
    nab = din("nab", [depth, 4, 5, 128, 640])
    w_out = din("w_out", [depth, D, D]); ln1 = din("ln1", [depth, 128, 16]); ln2 = din("ln2", [depth, 128, 16])
    rw = din("rw", [depth, D, NE]); rb = din("rb", [depth, NE])
    wgu = din("wgu", [depth, NE, 16, 128, 1024]); bguT = din("bguT", [depth, 128, NE * 16])
    wdn = din("wdn", [depth, NE, 8, 128, 1024]); bdn = din("bdn", [depth, NE, D])
    ident = din("ident", [128, 128])
    yT = nc.dram_tensor("yT", [D, T], F32, kind="ExternalOutput").ap()
    o_scr = nc.dram_tensor("o_scr", [4, 128, S], BF16).ap()
    og = nc.dram_tensor("og", [4, 2, 128, S], BF16).ap()
    x1_scr = nc.dram_tensor("x1_scr", [D, T], F32).ap()
    xb_scr = nc.dram_tensor("xb_scr", [4, 256, T], BF16).ap()
    xg = nc.dram_tensor("xg", [4, 2, 2, 128, T], BF16).ap()

    semstack = contextlib.ExitStack()
    nblk = T // SBK
    for l in range(depth):
        if l == 0:
            x_src = lambda tb: xT_full[:, tb * SBK:(tb + 1) * SBK].rearrange("(kc p) t -> p kc t", p=128)
        else:
            x_src = lambda tb, j: xg[:, tb // nblk, j, :, (tb % nblk) * SBK:(tb % nblk + 1) * SBK].rearrange(
                "i p t -> p i t")
        attn_phase(nc, semstack, "a%d_" % l, dict(
            w_sel=w_sel[l], t5c=t5c, t5b=t5b, lamv=lamv[l:l + 1, :], cst=cst[l], gsub=gsub[l], nab=nab[l],
            o_scr=o_scr, x_src=x_src, x_3d=(l == 0)), S=S)
        xchg_phase(nc, semstack, "xo%d" % l,
                   [(o_scr[i], og[i].rearrange("r p t -> (r p) t")) for i in range(4)])
        last = (l == depth - 1)
        moe_phase(nc, semstack, "m%d_" % l, dict(
            og=og, xres=(xT_own if l == 0 else x1_scr), w_out=w_out[l], ln1=ln1[l], ln2=ln2[l], rw=rw[l],
            rb=rb[l:l + 1, :], wgu=wgu[l], bguT=bguT[l], wdn=wdn[l], bdn=bdn[l], ident=ident, smask=smask,
            y_f32=(yT if last else x1_scr), y_b16=(None if last else xb_scr)), T=T, NE=NE, TB=TB)
        if not last:
            xchg_phase(nc, semstack, "xx%d" % l,
                       [(xb_scr[i], xg[i].rearrange("r j p t -> (r j p) t")) for i in range(4)])
    semstack.close()
    return nc


def _relayout_gu(w):
    L, E = w.shape[0], w.shape[1]
    v = w.reshape(L, E, 8, 128, 16, 128)
    order = [j for fc in range(8) for j in (fc, 8 + fc)]
    out = np.empty((L, E, 16, 128, 8, 128), np.float32)
    for ui, j in enumerate(order):
        out[:, :, ui] = v[:, :, :, :, j, :].transpose(0, 1, 3, 2, 4)
    return out.reshape(L, E, 16, 128, 1024)


def _relayout_dn(w):
    L, E = w.shape[0], w.shape[1]
    v = w.reshape(L, E, 8, 128, 8, 128)
    return np.ascontiguousarray(v.transpose(0, 1, 4, 3, 2, 5)).reshape(L, E, 8, 128, 1024)


def _wout_perm():
    rows = []
    for i in range(4):
        for r in range(2):
            st = [(2 * r) * 128, (2 * r + 1) * 128, 512 + (4 * r) * 64, 512 + (4 * r + 2) * 64][i]
            rows += list(range(st, st + 128))
    return rows


def make_in_maps(x, w_in, w_out, lambda_q1, lambda_k1, lambda_q2, lambda_k2, subln_g, t5_table, na_rpb,
                 ln1_g, ln1_b, router_w, router_b, w_gate_up, b_gate_up, w_down, b_down, ln2_g, ln2_b):
    f = np.float32
    A = lambda a: np.asarray(a, dtype=f)
    x = A(x)
    B_, S_, _ = x.shape
    depth = w_in.shape[0]
    NE = router_w.shape[-1]
    T = S_ // 2
    w_in = A(w_in)
    perm = _wout_perm()
    common = dict(
        w_out=np.ascontiguousarray(A(w_out)[:, perm, :]),
        ln1=np.stack([_pack_gb(A(ln1_g)[l], A(ln1_b)[l]) for l in range(depth)]),
        ln2=np.stack([_pack_gb(A(ln2_g)[l], A(ln2_b)[l]) for l in range(depth)]),
        rw=A(router_w), rb=A(router_b), wgu=_relayout_gu(A(w_gate_up)),
        bguT=np.stack([np.ascontiguousarray(A(b_gate_up)[l].reshape(NE, 16, 128).transpose(2, 0, 1).reshape(128, NE * 16))
                       for l in range(depth)]),
        wdn=_relayout_dn(A(w_down)), bdn=A(b_down), ident=np.eye(128, dtype=f))
    in_maps = []
    xTs = [np.ascontiguousarray(x[bi].T) for bi in range(B_)]
    for c in range(2 * B_):
        bi, r = c // 2, c % 2
        per = [prep_attn_inputs(x[bi][:8], w_in[l], r, l, A(lambda_q1)[l], A(lambda_k1)[l], A(lambda_q2)[l],
                                A(lambda_k2)[l], A(subln_g)[l], A(t5_table), A(na_rpb)[l], S=S_) for l in range(depth)]
        m = dict(common)
        m["xT_full"] = xTs[bi]
        m["xT_own"] = np.ascontiguousarray(xTs[bi][:, r * T:(r + 1) * T])
        sm = np.zeros((128, 2), f); sm[:, r] = 1.0
        m["smask"] = sm
        m["w_sel"] = np.stack([p["w_sel"] for p in per])
        m["t5c"] = per[0]["t5c"]; m["t5b"] = per[0]["t5b"]
        m["lamv"] = np.concatenate([p["lamv"] for p in per], axis=0)
        m["cst"] = np.stack([p["cst"] for p in per]); m["gsub"] = np.stack([p["gsub"] for p in per])
        m["nab"] = np.stack([p["nab"] for p in per])
        in_maps.append(m)
    return in_maps, (B_, S_, T)


def kernel(**inputs):
    in_maps, (B_, S_, T) = make_in_maps(**inputs)
    NE = np.asarray(inputs["router_w"]).shape[-1]
    depth = np.asarray(inputs["w_in"]).shape[0]
    nc = build_fused(S=S_, NE=NE, depth=depth)
    res = run_bass_kernel_spmd(nc, in_maps, core_ids=list(range(2 * B_)))
    out = np.empty((B_, S_, D), np.float32)
    for c in range(2 * B_):
        bi, r = c // 2, c % 2
        out[bi, r * T:(r + 1) * T, :] = res.results[c]["yT"].T
    return out
```
